# Optimizing a Trainium2 kernel written in Bass

```python
import math
import jax, jax.numpy as jnp
from jax import lax
import numpy as np

D_MODEL = 1024
BATCH = 2
SEQ = 8192
DEPTH = 2

GRID_W = 64
CTX_LEN = 256
EPS = 1e-6
NEG_INF = -1e30

NA_HEADS = 8
NA_HEAD_DIM = 64
NA_WIN_H = 8
NA_WIN_W = 16
NA_QBLOCK = NA_WIN_W
NA_KBLOCK = 2 * NA_WIN_W
NA_WIDTH = NA_HEADS * NA_HEAD_DIM
SG_GROUPS = 8
SG_GROUP_DIM = 64
SG_CHUNK = 128
SG_WIDTH = SG_GROUPS * SG_GROUP_DIM
AB_IN = 3 * NA_WIDTH + 2 * SG_WIDTH
AB_OUT = NA_WIDTH + SG_WIDTH

RET_HEADS = 4
RET_QK_DIM = 256
RET_V_DIM = 512
RET_CHUNK = 128
RET_QK_WIDTH = RET_HEADS * RET_QK_DIM
RET_V_WIDTH = RET_HEADS * RET_V_DIM
RET_IN = 2 * RET_QK_WIDTH + 2 * RET_V_WIDTH
ROPE_BASE = 10000.0

N_EXPERTS = 16
EC_CAPACITY = 2
D_FF_EXPERT = 2816

N_EVEN = (DEPTH + 1) // 2
N_ODD = DEPTH // 2

kernel_name = "hybrid_natten_sgu_retention_ecmoe_dit"

F32 = jnp.float32


def rmsnorm(x, g):
    xf = x.astype(F32)
    y = xf * lax.rsqrt(jnp.mean(xf * xf, axis=-1, keepdims=True) + EPS)
    return (y * g.astype(F32)).astype(x.dtype)


def adaln(cond, w, b):
    m = jax.nn.silu(cond) @ w + b
    return jnp.split(m[..., None, :], 6, axis=-1)


def modulate(h, shift, scale):
    return h * (1 + scale) + shift


def to_heads(t, n_heads):
    b, l, _ = t.shape
    return t.reshape(b, l, n_heads, -1).transpose(0, 2, 1, 3)


def from_heads(t):
    b, h, l, d = t.shape
    return t.transpose(0, 2, 1, 3).reshape(b, l, h * d)


def axial_rope_tables(rows, dim):
    axis_dim = dim // 2
    inv = 1.0 / (ROPE_BASE ** (jnp.arange(0, axis_dim, 2, dtype=F32) / axis_dim))
    t = jnp.arange(rows * GRID_W)
    r = (t // GRID_W).astype(F32)
    col = (t % GRID_W).astype(F32)
    ang = jnp.concatenate([r[:, None] * inv, col[:, None] * inv], axis=-1)
    return jnp.cos(ang), jnp.sin(ang)


def apply_rope(x, cos, sin):
    x1, x2 = jnp.split(x, 2, axis=-1)
    return jnp.concatenate([x1 * cos - x2 * sin, x1 * sin + x2 * cos], axis=-1).astype(x.dtype)


def dense_attention(q, k, v):
    s = jnp.einsum('bhqd,bhkd->bhqk', q, k).astype(F32) * (q.shape[-1] ** -0.5)
    p = jax.nn.softmax(s, axis=-1).astype(v.dtype)
    return jnp.einsum('bhqk,bhkd->bhqd', p, v)


def neighbourhood_attention(q, k, v, k_ctx, v_ctx, rpb):
    B, H, L, d = q.shape
    rows = L // GRID_W
    kh = min(NA_WIN_H, rows)
    ncb = GRID_W // NA_QBLOCK
    scale = d ** -0.5
    qcol = np.arange(GRID_W).reshape(ncb, NA_QBLOCK)
    cstart = np.clip(qcol - NA_WIN_W // 2, 0, GRID_W - NA_WIN_W)
    kc0 = np.minimum(cstart[:, 0], GRID_W - NA_KBLOCK)
    kcol = kc0[:, None] + np.arange(NA_KBLOCK)
    col_ok = (kcol[:, None, :] >= cstart[:, :, None]) & (kcol[:, None, :] < cstart[:, :, None] + NA_WIN_W)
    dcol = np.clip(kcol[:, None, :] - qcol[:, :, None], 1 - NA_WIN_W, NA_WIN_W - 1) + NA_WIN_W - 1
    mask = jnp.asarray(col_ok)[:, :, None, :]
    qg = q.reshape(B, H, rows, ncb, NA_QBLOCK, d)
    kg = k.reshape(B, H, rows, GRID_W, d)
    vg = v.reshape(B, H, rows, GRID_W, d)
    nk = kh * NA_KBLOCK

    def row_block(i):
        r0 = jnp.clip(i - kh // 2, 0, rows - kh)
        k_blk = lax.dynamic_slice_in_dim(kg, r0, kh, axis=2)[:, :, :, kcol]
        v_blk = lax.dynamic_slice_in_dim(vg, r0, kh, axis=2)[:, :, :, kcol]
        q_row = lax.dynamic_index_in_dim(qg, i, axis=2, keepdims=False)
        s_nb = jnp.einsum('bhcqd,bhrckd->bhcqrk', q_row, k_blk).astype(F32) * scale
        drow = r0 + jnp.arange(kh) - i + NA_WIN_H - 1
        bias = rpb[:, drow][:, :, dcol].astype(F32)
        s_nb = jnp.where(mask, s_nb + jnp.transpose(bias, (0, 2, 3, 1, 4)), NEG_INF)
        s_c = jnp.einsum('bhcqd,bhkd->bhcqk', q_row, k_ctx).astype(F32) * scale
        s = jnp.concatenate([s_nb.reshape(B, H, ncb, NA_QBLOCK, nk), s_c], axis=-1)
        p = jax.nn.softmax(s, axis=-1).astype(v.dtype)
        p_nb = p[..., :nk].reshape(B, H, ncb, NA_QBLOCK, kh, NA_KBLOCK)
        p_c = p[..., nk:]
        return (jnp.einsum('bhcqrk,bhrckd->bhcqd', p_nb, v_blk)
                + jnp.einsum('bhcqk,bhkd->bhcqd', p_c, v_ctx))

    out = lax.map(row_block, jnp.arange(rows))
    return jnp.moveaxis(out, 0, 2).reshape(B, H, L, d)


def spatial_gating(u, v, norm_g, w_s, b_s):
    b, l, _ = v.shape
    vn = rmsnorm(v, norm_g).reshape(b, l // SG_CHUNK, SG_CHUNK, SG_GROUPS, SG_GROUP_DIM)
    mixed = jnp.einsum('gpq,bnqgc->bnpgc', w_s, vn) + b_s.T[:, :, None]
    return u * mixed.reshape(b, l, SG_WIDTH)


def na_sgu_mixer(h, h_ctx, w_in, w_out, rpb, sgu_g, sgu_w, sgu_b, with_ctx_out):
    o1, o3, o4 = NA_WIDTH, 3 * NA_WIDTH, 3 * NA_WIDTH + SG_WIDTH
    z = h @ w_in
    q = to_heads(z[..., :o1], NA_HEADS)
    k = to_heads(z[..., o1:2 * o1], NA_HEADS)
    v = to_heads(z[..., 2 * o1:o3], NA_HEADS)
    u = jax.nn.gelu(z[..., o3:o4])
    gv = jax.nn.gelu(z[..., o4:])
    zc_kv = h_ctx @ w_in[:, o1:o3]
    k_c = to_heads(zc_kv[..., :NA_WIDTH], NA_HEADS)
    v_c = to_heads(zc_kv[..., NA_WIDTH:], NA_HEADS)
    a = from_heads(neighbourhood_attention(q, k, v, k_c, v_c, rpb))
    bsg = spatial_gating(u, gv, sgu_g, sgu_w, sgu_b)
    y = jnp.concatenate([a, bsg], axis=-1) @ w_out
    if not with_ctx_out:
        return y, None
    q_c = to_heads(h_ctx @ w_in[:, :o1], NA_HEADS)
    zc_ug = jax.nn.gelu(h_ctx @ w_in[:, o3:])
    a_c = from_heads(dense_attention(q_c, k_c, v_c))
    b_c = spatial_gating(zc_ug[..., :SG_WIDTH], zc_ug[..., SG_WIDTH:], sgu_g, sgu_w, sgu_b)
    y_c = jnp.concatenate([a_c, b_c], axis=-1) @ w_out
    return y, y_c


def retention_scan(q, k, v, log_gamma, s0):
    B, H, L, dk = q.shape
    dv = v.shape[-1]
    C = RET_CHUNK
    n = L // C
    chunks = lambda t: jnp.moveaxis(t.astype(F32).reshape(B, H, n, C, t.shape[-1]), 2, 0)
    idx = jnp.arange(C, dtype=F32)
    diff = idx[:, None] - idx[None, :]
    decay_in = jnp.where(diff >= 0, jnp.exp(jnp.maximum(diff, 0.0) * log_gamma[:, None, None]), 0.0)
    q_decay = jnp.exp((idx + 1) * log_gamma[:, None])[:, :, None]
    k_decay = jnp.exp((C - 1 - idx) * log_gamma[:, None])[:, :, None]
    chunk_decay = jnp.exp(C * log_gamma)[:, None, None]

    def step(s, inp):
        qj, kj, vj = inp
        att = jnp.einsum('bhqd,bhkd->bhqk', qj, kj) * decay_in
        o = jnp.einsum('bhqk,bhkv->bhqv', att, vj) + jnp.einsum('bhqd,bhdv->bhqv', qj, s) * q_decay
        s_new = s * chunk_decay + jnp.einsum('bhkd,bhkv->bhdv', kj * k_decay, vj)
        return s_new, o

    s_fin, o = lax.scan(step, s0, (chunks(q), chunks(k), chunks(v)))
    return jnp.moveaxis(o, 0, 2).reshape(B, H, L, dv), s_fin


def retention_final_state(k, v, log_gamma):
    L = k.shape[2]
    w = jnp.exp((L - 1 - jnp.arange(L, dtype=F32))[None, :] * log_gamma[:, None])
    return jnp.einsum('bhld,bhlv,hl->bhdv', k.astype(F32), v.astype(F32), w)


def retention_output(o, g, w_out):
    of = o.astype(F32)
    of = of * lax.rsqrt(jnp.mean(of * of, axis=-1, keepdims=True) + EPS)
    y = from_heads(of.astype(g.dtype))
    return (jax.nn.silu(g) * y) @ w_out


def retention_mixer(h, h_ctx, w_in, w_out, decay_logit, cos, sin, with_ctx_out):
    o1, o2, o3 = RET_QK_WIDTH, 2 * RET_QK_WIDTH, 2 * RET_QK_WIDTH + RET_V_WIDTH
    kscale = RET_QK_DIM ** -0.5
    z = h @ w_in
    q = apply_rope(to_heads(z[..., :o1], RET_HEADS), cos, sin)
    k = apply_rope(to_heads(z[..., o1:o2], RET_HEADS), cos, sin) * kscale
    v = to_heads(z[..., o2:o3], RET_HEADS)
    g = z[..., o3:]
    zc_kv = h_ctx @ w_in[:, o1:o3]
    k_c = to_heads(zc_kv[..., :RET_QK_WIDTH], RET_HEADS) * kscale
    v_c = to_heads(zc_kv[..., RET_QK_WIDTH:], RET_HEADS)
    if with_ctx_out:
        q_c = to_heads(h_ctx @ w_in[:, :o1], RET_HEADS)
        g_c = h_ctx @ w_in[:, o3:]
    log_gamma = jax.nn.log_sigmoid(decay_logit.astype(F32))
    bc = h_ctx.shape[0]
    lat_outs = []
    ctx_outs = []
    for direction in range(2):
        f = (lambda t: jnp.flip(t, axis=2)) if direction == 1 else (lambda t: t)
        lg = log_gamma[direction]
        if with_ctx_out:
            s0 = jnp.zeros((bc, RET_HEADS, RET_QK_DIM, RET_V_DIM), F32)
            oc, s_c = retention_scan(f(q_c), f(k_c), f(v_c), lg, s0)
            ctx_outs.append(f(oc))
        else:
            s_c = retention_final_state(f(k_c), f(v_c), lg)
        ol, _ = retention_scan(f(q), f(k), f(v), lg, s_c)
        lat_outs.append(f(ol))
    y = retention_output(lat_outs[0] + lat_outs[1], g, w_out)
    if not with_ctx_out:
        return y, None
    y_c = retention_output(ctx_outs[0] + ctx_outs[1], g_c, w_out)
    return y, y_c


def expert_choice_ffn(h, w_router, w_gate, w_up, w_down):
    B, N, _ = h.shape
    cap = EC_CAPACITY * N // N_EXPERTS
    aff = jax.nn.softmax(jnp.einsum('bnd,de->bne', h, w_router).astype(F32), axis=-1)
    gate, idx = lax.top_k(jnp.swapaxes(aff, 1, 2), cap)
    bidx = jnp.arange(B)[:, None, None]
    xe = h[bidx, idx]
    hid = jax.nn.silu(jnp.einsum('becd,edf->becf', xe, w_gate)) * jnp.einsum('becd,edf->becf', xe, w_up)
    ye = jnp.einsum('becf,efd->becd', hid, w_down) * gate[..., None].astype(h.dtype)
    return jnp.zeros_like(h).at[bidx, idx].add(ye)


def setup_inputs(seed: int = 0) -> dict:
    key = jax.random.key(seed)
    ks = jax.random.split(key, 24)
    nrm = lambda k, shape, fan_in: jax.random.normal(k, shape, F32) * (fan_in ** -0.5)
    noise = lambda k, shape, s: jax.random.normal(k, shape, F32) * s
    base_logit = jnp.log(2.0 ** (5.0 + jnp.arange(RET_HEADS, dtype=F32)) - 1.0)
    return {
        "x": jax.random.normal(ks[0], (BATCH, SEQ, D_MODEL), F32),
        "c": jax.random.normal(ks[1], (BATCH, D_MODEL), F32),
        "ctx": jax.random.normal(ks[2], (BATCH, CTX_LEN, D_MODEL), F32),
        "c_ctx": jax.random.normal(ks[3], (D_MODEL,), F32),
        "ada_w": nrm(ks[4], (DEPTH, D_MODEL, 6 * D_MODEL), D_MODEL),
        "ada_b": noise(ks[5], (DEPTH, 6 * D_MODEL), 0.01),
        "norm1_g": 1.0 + noise(ks[6], (DEPTH, D_MODEL), 0.02),
        "norm2_g": 1.0 + noise(ks[7], (DEPTH, D_MODEL), 0.02),
        "ab_w_in": nrm(ks[8], (N_EVEN, D_MODEL, AB_IN), D_MODEL),
        "ab_w_out": nrm(ks[9], (N_EVEN, AB_OUT, D_MODEL), AB_OUT),
        "na_rpb": noise(ks[10], (N_EVEN, NA_HEADS, 2 * NA_WIN_H - 1, 2 * NA_WIN_W - 1), 0.02),
        "sgu_norm_g": 1.0 + noise(ks[11], (N_EVEN, SG_WIDTH), 0.02),
        "sgu_w": nrm(ks[12], (N_EVEN, SG_GROUPS, SG_CHUNK, SG_CHUNK), SG_CHUNK),
        "sgu_b": 1.0 + noise(ks[13], (N_EVEN, SG_GROUPS, SG_CHUNK), 0.01),
        "ret_w_in": nrm(ks[14], (N_ODD, D_MODEL, RET_IN), D_MODEL),
        "ret_w_out": nrm(ks[15], (N_ODD, RET_V_WIDTH, D_MODEL), RET_V_WIDTH),
        "ret_decay_logit": base_logit + noise(ks[16], (N_ODD, 2, RET_HEADS), 0.05),
        "moe_router": nrm(ks[17], (DEPTH, D_MODEL, N_EXPERTS), D_MODEL),
        "moe_w_gate": nrm(ks[18], (DEPTH, N_EXPERTS, D_MODEL, D_FF_EXPERT), D_MODEL),
        "moe_w_up": nrm(ks[19], (DEPTH, N_EXPERTS, D_MODEL, D_FF_EXPERT), D_MODEL),
        "moe_w_down": nrm(ks[20], (DEPTH, N_EXPERTS, D_FF_EXPERT, D_MODEL), D_FF_EXPERT),
        "final_norm_g": 1.0 + noise(ks[21], (D_MODEL,), 0.02),
    }


def reference(x, c, ctx, c_ctx, ada_w, ada_b, norm1_g, norm2_g, ab_w_in, ab_w_out, na_rpb,
              sgu_norm_g, sgu_w, sgu_b, ret_w_in, ret_w_out, ret_decay_logit,
              moe_router, moe_w_gate, moe_w_up, moe_w_down, final_norm_g):
    L = x.shape[1]
    rows = L // GRID_W
    cos, sin = axial_rope_tables(rows, RET_QK_DIM)
    xc = ctx
    for layer in range(DEPTH):
        last = layer == DEPTH - 1
        sh1, sc1, g1, sh2, sc2, g2 = adaln(c, ada_w[layer], ada_b[layer])
        csh1, csc1, cg1, csh2, csc2, cg2 = adaln(c_ctx, ada_w[layer], ada_b[layer])
        h = modulate(rmsnorm(x, norm1_g[layer]), sh1, sc1)
        hc = modulate(rmsnorm(xc, norm1_g[layer]), csh1, csc1)
        if layer % 2 == 0:
            j = layer // 2
            y, y_c = na_sgu_mixer(h, hc, ab_w_in[j], ab_w_out[j], na_rpb[j],
                                  sgu_norm_g[j], sgu_w[j], sgu_b[j], not last)
        else:
            j = layer // 2
            y, y_c = retention_mixer(h, hc, ret_w_in[j], ret_w_out[j], ret_decay_logit[j],
                                     cos, sin, not last)
        x = x + g1 * y
        h2 = modulate(rmsnorm(x, norm2_g[layer]), sh2, sc2)
        x = x + g2 * expert_choice_ffn(h2, moe_router[layer], moe_w_gate[layer],
                                       moe_w_up[layer], moe_w_down[layer])
        if not last:
            xc = xc + cg1 * y_c
            hc2 = modulate(rmsnorm(xc, norm2_g[layer]), csh2, csc2)
            xc = xc + cg2 * expert_choice_ffn(hc2, moe_router[layer], moe_w_gate[layer],
                                              moe_w_up[layer], moe_w_down[layer])
    return rmsnorm(x, final_norm_g)
```

```python
import numpy as np
import ml_dtypes
from contextlib import ExitStack
import concourse.bass as bass
import concourse.mybir as mybir
from concourse.bass_utils import run_bass_kernel_spmd

F32 = mybir.dt.float32
BF16 = mybir.dt.bfloat16
I32 = mybir.dt.int32
U32 = mybir.dt.uint32
ALU = mybir.AluOpType
AF = mybir.ActivationFunctionType
AX = mybir.AxisListType
NEG = -1.0e30
BIG = 1.0e6
NPBF = ml_dtypes.bfloat16


class Res:
    def __init__(self, name, t=None):
        self.name = name
        self.t = t
        self.w = None
        self.r = {}
        self.acc = False
        self.wl = []

    def __getitem__(self, idx):
        return self.t[idx]


class Prog:
    def __init__(self, nc, es, n_dma_sems=8):
        self.nc = nc
        self.es = es
        self.names = ('pe', 'dve', 'act', 'pool', 'sp')
        self.sems = {}
        self.cnt = {}
        self.ops = {k: [] for k in self.names}
        for k in self.names:
            self.sems[k] = es.enter_context(nc.semaphore('s_' + k))
            self.cnt[k] = 0
        self.waited = {k: {} for k in self.names}
        self.dma_ring = {}
        for q in ('sp', 'act', 'pool'):
            ring = []
            for i in range(n_dma_sems):
                key = 'd_%s%d' % (q, i)
                self.sems[key] = es.enter_context(nc.semaphore(key))
                self.cnt[key] = 0
                ring.append(key)
            self.dma_ring[q] = [ring, 0]
        self.same_engine_sync = True

    def reg(self, eng, val):
        key = ('reg', val)
        if key not in self.__dict__.setdefault('_regs', {}):
            self._regs[key] = eng.to_reg(val)
        return self._regs[key]

    def sb(self, name, shape, dt):
        t = self.es.enter_context(self.nc.sbuf_tensor('sb_' + name, list(shape), dt))
        return Res(name, t)

    def sbn(self, name, shape, dt, n):
        return [self.sb('%s_%d' % (name, i), shape, dt) for i in range(n)]

    def ps(self, name, shape, dt):
        t = self.es.enter_context(self.nc.psum_tensor('ps_' + name, list(shape), dt))
        return Res(name, t)

    def dram(self, name, shape, dt, kind):
        t = self.nc.dram_tensor(name, list(shape), dt, kind=kind)
        return Res(name, t)

    def _deps(self, reads, writes):
        deps = []
        for r in reads:
            if r.w is not None:
                deps.append(r.w)
            deps.extend(r.wl)
        for w in writes:
            if w.w is not None and not w.acc:
                deps.append(w.w)
            if not w.acc:
                deps.extend(w.wl)
            deps.extend(w.r.items())
        return deps

    def _waits(self, e, deps):
        need = {}
        for k, v in deps:
            if v > need.get(k, 0):
                need[k] = v
        out = []
        for k, v in need.items():
            if k == e and (e == 'pe' or not self.same_engine_sync):
                continue
            if self.waited[e].get(k, 0) >= v:
                continue
            self.waited[e][k] = v
            out.append((k, v))
        return out

    def _mark(self, me, reads, writes):
        k, v = me
        for r in reads:
            if r.r.get(k, 0) < v:
                r.r[k] = v
        for w in writes:
            if w.acc:
                w.wl.append(me)
            else:
                w.w = me
                w.wl = []
                w.r = {}

    def op(self, e, fn, reads=(), writes=()):
        waits = self._waits(e, self._deps(reads, writes))
        self.cnt[e] += 1
        sems = self.sems

        def run(eng, waits=waits, fn=fn, e=e):
            for k, v in waits:
                eng.wait_ge(sems[k], v)
            fn(eng).then_inc(sems[e], 1)
        self.ops[e].append(run)
        self._mark((e, self.cnt[e]), reads, writes)

    def dma(self, q, fn, reads=(), writes=()):
        ring, idx = self.dma_ring[q]
        key = ring[idx % len(ring)]
        self.dma_ring[q][1] += 1
        deps = self._deps(reads, writes)
        if self.cnt[key] > 0:
            deps.append((key, self.cnt[key]))
        waits = self._waits(q, deps)
        self.cnt[key] += 16
        sems = self.sems

        def run(eng, waits=waits, fn=fn, key=key):
            for k, v in waits:
                eng.wait_ge(sems[k], v)
            fn(eng).then_inc(sems[key], 16)
        self.ops[q].append(run)
        self._mark((key, self.cnt[key]), reads, writes)

    def finish(self):
        finals = [(k, v) for k, v in self.cnt.items() if v > 0]
        sems = self.sems

        def run(eng):
            for k, v in finals:
                eng.wait_ge(sems[k], v)
        self.ops['sp'].append(run)
        nc = self.nc
        ops = self.ops
        with nc.Block() as blk:
            @blk.tensor
            def _(e):
                for f in ops['pe']:
                    f(e)

            @blk.vector
            def _(e):
                for f in ops['dve']:
                    f(e)

            @blk.scalar
            def _(e):
                for f in ops['act']:
                    f(e)

            @blk.gpsimd
            def _(e):
                for f in ops['pool']:
                    f(e)

            @blk.sync
            def _(e):
                for f in ops['sp']:
                    f(e)


def wview(d, p=128):
    return d.t.rearrange("(c p) n -> p c n", p=p)


def make_consts(P):
    C = {}
    identf = P.sb("identf", [128, 128], F32)
    ident = P.sb("ident", [128, 128], BF16)
    ones = P.sb("ones_f", [128, 128], F32)
    P.op('pool', lambda e: e.memset(identf[:], 0.0), writes=[identf])
    P.op('pool', lambda e: e.affine_select(out=identf[:], in_=identf[:], pattern=[[-1, 128]], compare_op=ALU.not_equal,
                                          fill=1.0, base=0, channel_multiplier=1), reads=[identf], writes=[identf])
    P.op('dve', lambda e: e.tensor_copy(out=ident[:], in_=identf[:]), reads=[identf], writes=[ident])
    P.op('pool', lambda e: e.memset(ones[:], 1.0), writes=[ones])
    C['identf'] = identf
    C['ident'] = ident
    C['ones'] = ones
    return C


def emit_adaln(P, C, condT_d, adaw_d, adab_d, col0, nblk, psum, tag):
    ones = C['ones']
    condT = P.sb(tag + "condT", [128, 16], F32)
    sl = P.sb(tag + "silu", [128, 16], F32)
    bc = P.sb(tag + "bc", [128, 16, 128], F32)
    ncols = nblk * 1024
    adabs = P.sbn(tag + "adab", [128, 512], F32, 2)
    wbuf = P.sbn(tag + "adaw", [128, 8, 512], F32, 1)
    mod = [[P.sb("%smod%d_%d" % (tag, s, k), [128, 1024], F32) for k in range(nblk)] for s in range(2)]
    P.dma('sp', lambda e: e.dma_start(out=condT[:], in_=condT_d[:, :]), reads=[condT_d], writes=[condT])
    P.op('act', lambda e: e.activation(out=sl[:], in_=condT[:], func=AF.Silu), reads=[condT], writes=[sl])
    for i in range(16):
        P.op('dve', lambda e, i=i: e.tensor_scalar(out=bc[:, i, :], in0=ones[:], scalar1=sl[:, i:i + 1], scalar2=None, op0=ALU.mult),
             reads=[ones, sl], writes=[bc])
    wv = wview(adaw_d)
    for cg in range(ncols // 512):
        wb = wbuf[0]
        c0 = col0 + cg * 512
        adab = adabs[cg % 2]
        P.dma('sp', lambda e, adab=adab, c0=c0: e.dma_start(out=adab[:], in_=adab_d[0:1, c0:c0 + 512].to_broadcast([128, 512])), reads=[adab_d], writes=[adab])
        P.dma('sp', lambda e, wb=wb, c0=c0: e.dma_start(out=wb[:], in_=wv[:, :, c0:c0 + 512]), reads=[adaw_d], writes=[wb])
        for s in range(2):
            ps = psum[(cg * 2 + s) % len(psum)]
            for c in range(8):
                P.op('pe', lambda e, ps=ps, s=s, c=c, wb=wb: e.matmul(ps[:, 0:512], lhsT=bc[:, s * 8 + c, :], rhs=wb[:, c, :], start=(c == 0), stop=(c == 7)),
                     reads=[bc, wb], writes=[ps])
            dst = mod[s][cg // 2]
            P.op('dve', lambda e, ps=ps, dst=dst, cg=cg, adab=adab: e.tensor_tensor(out=dst[:, (cg % 2) * 512:(cg % 2) * 512 + 512], in0=ps[:, 0:512], in1=adab[:, :], op=ALU.add),
                 reads=[ps, adab], writes=[dst])
    return mod


def emit_rstd(P, x, junk, ss, rstd, D, eps=1e-6):
    P.op('act', lambda e: e.activation(out=junk[:, 0:D], in_=x, func=AF.Square, accum_out=ss[:]), reads=[x.res] if hasattr(x, 'res') else [], writes=[junk, ss])


class W:
    def __init__(self, ap, res):
        self.ap = ap
        self.res = res


def emit_norm_mod(P, xres, xap, gs, sh, outres, outap, junk, ss, rstd, D=1024):
    P.op('act', lambda e: e.activation(out=junk[:, 0:D], in_=xap, func=AF.Square, accum_out=ss[:]), reads=[xres], writes=[junk, ss])
    P.op('dve', lambda e: e.tensor_scalar(out=rstd[:], in0=ss[:], scalar1=1.0 / D, scalar2=1e-6, op0=ALU.mult, op1=ALU.add), reads=[ss], writes=[rstd])
    P.op('act', lambda e: e.activation(out=rstd[:], in_=rstd[:], func=AF.Sqrt), reads=[rstd], writes=[rstd])
    P.op('dve', lambda e: e.reciprocal(out=rstd[:], in_=rstd[:]), reads=[rstd], writes=[rstd])
    if sh is None:
        P.op('dve', lambda e: e.scalar_tensor_tensor(out=outap, in0=xap, scalar=rstd[:, 0:1], in1=gs[:, 0:D], op0=ALU.mult, op1=ALU.mult),
             reads=[xres, rstd, gs], writes=[outres])
    else:
        P.op('dve', lambda e: e.scalar_tensor_tensor(out=junk[:, 0:D], in0=xap, scalar=rstd[:, 0:1], in1=gs[:, 0:D], op0=ALU.mult, op1=ALU.mult),
             reads=[xres, rstd, gs], writes=[junk])
        P.op('dve', lambda e: e.tensor_tensor(out=outap, in0=junk[:, 0:D], in1=sh[:, 0:D], op=ALU.add), reads=[junk, sh], writes=[outres])


def emit_T(P, C, src, dst, psT, nchunk=8, rows=128, evac='act', f32=False):
    ident = C['identf'] if f32 else C['ident']
    for c in range(nchunk):
        P.op('pe', lambda e, c=c: e.transpose(out=psT[:, c, 0:rows], in_=src[0:rows, c * 128:(c + 1) * 128], identity=ident[0:rows, 0:rows]),
             reads=[src, ident], writes=[psT])
    if evac == 'act':
        P.op('act', lambda e: e.copy(out=dst[:, 0:nchunk, 0:rows], in_=psT[:, 0:nchunk, 0:rows]), reads=[psT], writes=[dst])
    else:
        P.op('dve', lambda e: e.tensor_copy(out=dst[:, 0:nchunk, 0:rows], in_=psT[:, 0:nchunk, 0:rows]), reads=[psT], writes=[dst])


def emit_gelu(P, src, dst, t1, W_):
    P.op('act', lambda e: e.activation(out=t1[:, 0:W_], in_=src[:, 0:W_], func=AF.Square), reads=[src], writes=[t1])
    P.op('dve', lambda e: e.tensor_scalar(out=t1[:, 0:W_], in0=t1[:, 0:W_], scalar1=0.044715, scalar2=1.0, op0=ALU.mult, op1=ALU.add), reads=[t1], writes=[t1])
    P.op('dve', lambda e: e.tensor_tensor(out=t1[:, 0:W_], in0=t1[:, 0:W_], in1=src[:, 0:W_], op=ALU.mult), reads=[t1, src], writes=[t1])
    P.op('act', lambda e: e.activation(out=t1[:, 0:W_], in_=t1[:, 0:W_], func=AF.Sigmoid, scale=1.5957691216057308), reads=[t1], writes=[t1])
    P.op('dve', lambda e: e.tensor_tensor(out=dst[:, 0:W_], in0=t1[:, 0:W_], in1=src[:, 0:W_], op=ALU.mult), reads=[t1, src], writes=[dst])


NTOK = 8448
NT = 66


def na_variant(t):
    if t == 0:
        return 0, 'e0'
    if t == 1:
        return 0, 'e2'
    if t == 62:
        return 120, 'e124'
    if t == 63:
        return 120, 'e126'
    return 2 * t - 4, 'int'


NA_VARS = {
    'int': ((0, 3), (1, 2)),
    'e0': ((0, 7), (0, 6)),
    'e2': ((0, 5), (0, 4)),
    'e124': ((0, 3), (0, 2)),
    'e126': ((0, 1), (0, 0)),
}


def build_p1(stage=3):
    nc = bass.Bass("TRN2", target_bir_lowering=False)
    with ExitStack() as es:
        P = Prog(nc, es)
        Din = lambda n, s, dt=F32: P.dram(n, s, dt, "ExternalInput")
        xs_d = Din("xs", [NTOK, 1024])
        condT_d = Din("condT", [128, 16])
        adaw_d = Din("adaw", [1024, 6144])
        adab_d = Din("adab", [1, 6144])
        n1g_d = Din("n1g", [1, 1024])
        wqkv_d = Din("wqkv", [1024, 384])
        Rb_d = Din("Rb", [128, 2, 960])
        xq_d = Din("xq", [2304, 1024])
        wug_d = Din("wug", [1024, 1024])
        sgug_d = Din("sgug", [1, 512])
        swT_d = Din("swT", [128, 8, 128])
        sbT_d = Din("sbT", [128, 8])
        a_d = P.dram("a_out", [NTOK, 128], BF16, "ExternalOutput")
        bsg_d = P.dram("bsg_out", [2304, 512], BF16, "ExternalOutput")

        C = make_consts(P)
        psS = [P.ps("psS%d" % i, [128, 1024], F32) for i in range(2)]
        psT = [P.ps("psT%d" % i, [128, 8, 128], BF16) for i in range(2)]
        psO = [P.ps("psO%d" % i, [128, 512], F32) for i in range(2)]

        mod = emit_adaln(P, C, condT_d, adaw_d, adab_d, 0, 2, psO, "a")
        n1g = P.sb("n1g", [128, 1024], F32)
        P.dma('sp', lambda e: e.dma_start(out=n1g[:], in_=n1g_d[0:1, :].to_broadcast([128, 1024])), reads=[n1g_d], writes=[n1g])
        sh = [mod[0][0], mod[1][0]]
        gs = [mod[0][1], mod[1][1]]
        for s in range(2):
            P.op('dve', lambda e, s=s: e.scalar_tensor_tensor(out=gs[s][:], in0=gs[s][:], scalar=1.0, in1=n1g[:], op0=ALU.add, op1=ALU.mult),
                 reads=[gs[s], n1g], writes=[gs[s]])

        if stage < 1:
            P.finish()
            return nc
        wqkv = P.sb("wqkv", [128, 8, 384], BF16)
        P.dma('pool', lambda e: e.dma_start(out=wqkv[:], in_=wview(wqkv_d)), reads=[wqkv_d], writes=[wqkv])
        wug = P.sb("wug", [128, 8, 1024], BF16)
        P.dma('pool', lambda e: e.dma_start(out=wug[:], in_=wview(wug_d)), reads=[wug_d], writes=[wug])
        swT = P.sb("swT", [128, 8, 128], BF16)
        P.dma('pool', lambda e: e.dma_start(out=swT[:], in_=swT_d[:, :, :]), reads=[swT_d], writes=[swT])
        sbT = P.sb("sbT", [128, 8], F32)
        P.dma('sp', lambda e: e.dma_start(out=sbT[:], in_=sbT_d[:, :]), reads=[sbT_d], writes=[sbT])
        sgug = P.sb("sgug", [128, 512], F32)
        P.dma('sp', lambda e: e.dma_start(out=sgug[:], in_=sgug_d[0:1, :].to_broadcast([128, 512])), reads=[sgug_d], writes=[sgug])
        R2 = P.sb("R2", [128, 2, 960], F32)
        P.dma('sp', lambda e: e.dma_start(out=R2[:], in_=Rb_d[:, :, :]), reads=[Rb_d], writes=[R2])

        B2 = {}
        for name, halves in NA_VARS.items():
            b2 = P.sb("B2" + name, [128, 2, 576], F32)
            P.op('dve', lambda e, b2=b2: e.memset(b2[:], NEG), writes=[b2])
            for half, (s0, off) in enumerate(halves):
                pr = slice(half * 64, half * 64 + 64)
                P.op('dve', lambda e, b2=b2, pr=pr, s0=s0, off=off: e.tensor_copy(
                    out=b2[pr, :, s0 * 64:(s0 + 8) * 64], in_=R2[pr, :, (s0 + off) * 64:(s0 + off + 8) * 64]),
                    reads=[R2], writes=[b2])
            B2[name] = b2

        if stage < 1.5:
            P.finish()
            return nc
        qT_all = P.sb("qT_all", [128, NTOK], BF16)
        kT_all = P.sb("kT_all", [128, NTOK], BF16)
        v_tm = P.sb("v_tm", [128, NT, 128], BF16)
        xbuf = P.sbn("xbuf", [128, 1024], F32, 2)
        hb = P.sbn("hb", [128, 1024], BF16, 2)
        hT = P.sbn("hT", [128, 8, 128], BF16, 2)
        junk = P.sb("junk", [128, 1024], F32)
        ss = P.sbn("ss", [128, 1], F32, 2)
        rstd = P.sbn("rstd", [128, 1], F32, 2)

        for t in range(NT):
            s = 0 if t < 64 else 1
            xt = xbuf[t % 2]
            P.dma('sp', lambda e, xt=xt, t=t: e.dma_start(out=xt[:], in_=xs_d[t * 128:(t + 1) * 128, :]), reads=[xs_d], writes=[xt])
            emit_norm_mod(P, xt, xt[:], gs[s], sh[s], hb[t % 2], hb[t % 2][:], junk, ss[t % 2], rstd[t % 2])
            emit_T(P, C, hb[t % 2], hT[t % 2], psT[t % 2])
            ht = hT[t % 2]
            pq, pk, pv = psO[0], psO[1], psS[t % 2]
            for c in range(8):
                P.op('pe', lambda e, c=c, pq=pq, ht=ht: e.matmul(pq[:, 0:128], lhsT=wqkv[:, c, 0:128], rhs=ht[:, c, :], start=(c == 0), stop=(c == 7)),
                     reads=[wqkv, ht], writes=[pq])
            P.op('act', lambda e, pq=pq, t=t: e.copy(out=qT_all[:, t * 128:(t + 1) * 128], in_=pq[:, 0:128]), reads=[pq], writes=[qT_all])
            for c in range(8):
                P.op('pe', lambda e, c=c, pk=pk, ht=ht: e.matmul(pk[:, 0:128], lhsT=wqkv[:, c, 128:256], rhs=ht[:, c, :], start=(c == 0), stop=(c == 7)),
                     reads=[wqkv, ht], writes=[pk])
            P.op('act', lambda e, pk=pk, t=t: e.copy(out=kT_all[:, t * 128:(t + 1) * 128], in_=pk[:, 0:128]), reads=[pk], writes=[kT_all])
            for c in range(8):
                P.op('pe', lambda e, c=c, pv=pv, ht=ht: e.matmul(pv[:, 0:128], lhsT=ht[:, c, :], rhs=wqkv[:, c, 256:384], start=(c == 0), stop=(c == 7)),
                     reads=[wqkv, ht], writes=[pv])
            P.op('dve', lambda e, pv=pv, t=t: e.tensor_copy(out=v_tm[:, t, :], in_=pv[:, 0:128]), reads=[pv], writes=[v_tm])

        if stage < 2:
            P.finish()
            return nc
        scb = P.sbn("scb", [128, 832], F32, 2)
        pbf = P.sbn("pbf", [128, 832], BF16, 2)
        PT = P.sbn("PT", [128, 7, 128], BF16, 2)
        negm = P.sbn("negm", [128, 1], F32, 2)
        rsum = P.sbn("rsum", [128, 1], F32, 2)
        a_t = P.sbn("a_t", [128, 128], BF16, 2)
        for t in range(NT):
            lat = t < 64
            f0, vname = na_variant(t) if lat else (0, None)
            at = a_t[t % 2]
            for hh in range(2):
                S = psS[hh]
                pr = slice(hh * 64, hh * 64 + 64)
                q_ap = qT_all[pr, t * 128:(t + 1) * 128]
                if lat:
                    k0 = f0 * 64
                    P.op('pe', lambda e, S=S, q_ap=q_ap, pr=pr, k0=k0: e.matmul(S[:, 0:512], lhsT=q_ap, rhs=kT_all[pr, k0:k0 + 512], start=True, stop=True),
                         reads=[qT_all, kT_all], writes=[S])
                    P.op('pe', lambda e, S=S, q_ap=q_ap, pr=pr, k0=k0: e.matmul(S[:, 512:576], lhsT=q_ap, rhs=kT_all[pr, k0 + 512:k0 + 576], start=True, stop=True),
                         reads=[qT_all, kT_all], writes=[S])
                P.op('pe', lambda e, S=S, q_ap=q_ap, pr=pr: e.matmul(S[:, 576:832], lhsT=q_ap, rhs=kT_all[pr, 8192:8448], start=True, stop=True),
                     reads=[qT_all, kT_all], writes=[S])
                sc = scb[hh]
                if lat:
                    b2 = B2[vname]
                    P.op('dve', lambda e, sc=sc, S=S, b2=b2, hh=hh: e.scalar_tensor_tensor(out=sc[:, 0:576], in0=S[:, 0:576], scalar=0.125, in1=b2[:, hh, :],
                                                                                  op0=ALU.mult, op1=ALU.add), reads=[S, b2], writes=[sc])
                    c0 = 0
                else:
                    c0 = 576
                P.op('act', lambda e, sc=sc, S=S: e.activation(out=sc[:, 576:832], in_=S[:, 576:832], func=AF.Copy, scale=0.125), reads=[S], writes=[sc])
                nm = negm[hh]
                rs = rsum[hh]
                P.op('dve', lambda e, sc=sc, nm=nm, c0=c0: e.tensor_reduce(out=nm[:], in_=sc[:, c0:832], axis=AX.X, op=ALU.max, negate=True), reads=[sc], writes=[nm])
                pb = pbf[hh]
                P.op('act', lambda e, sc=sc, nm=nm, rs=rs, pb=pb, c0=c0: e.activation(out=pb[:, c0:832], in_=sc[:, c0:832], func=AF.Exp, bias=nm[:, 0:1], scale=1.0, accum_out=rs[:]),
                     reads=[sc, nm], writes=[pb, rs])
                chunks = []
                if lat:
                    for c in range(4):
                        chunks.append((c, c * 128, 128, ('v', f0 // 2 + c)))
                    chunks.append((4, 512, 64, ('v', f0 // 2 + 4)))
                chunks.append((5, 576, 128, ('v', 64)))
                chunks.append((6, 704, 128, ('v', 65)))
                pt_ps = psT[hh]
                for (c, col, w, _) in chunks:
                    P.op('pe', lambda e, c=c, col=col, w=w, pb=pb, pt_ps=pt_ps: e.transpose(out=pt_ps[0:w, c, :], in_=pb[:, col:col + w], identity=C['ident'][:, :]),
                         reads=[pb, C['ident']], writes=[pt_ps])
                pt = PT[hh]
                cmin = chunks[0][0]
                P.op('act', lambda e, pt=pt, pt_ps=pt_ps, cmin=cmin: e.copy(out=pt[:, cmin:7, :], in_=pt_ps[:, cmin:7, :]), reads=[pt_ps], writes=[pt])
                po = psO[hh]
                for i, (c, col, w, (_, vt)) in enumerate(chunks):
                    P.op('pe', lambda e, c=c, w=w, vt=vt, pt=pt, po=po, hh=hh, i=i, n=len(chunks): e.matmul(
                        po[:, 0:64], lhsT=pt[0:w, c, :], rhs=v_tm[0:w, vt, hh * 64:(hh + 1) * 64], start=(i == 0), stop=(i == n - 1)),
                        reads=[pt, v_tm], writes=[po])
                P.op('dve', lambda e, rs=rs: e.reciprocal(out=rs[:], in_=rs[:]), reads=[rs], writes=[rs])
                P.op('dve', lambda e, at=at, po=po, rs=rs, hh=hh: e.tensor_scalar(out=at[:, hh * 64:(hh + 1) * 64], in0=po[:, 0:64], scalar1=rs[:, 0:1], scalar2=None, op0=ALU.mult),
                     reads=[po, rs], writes=[at])
            P.dma('sp', lambda e, at=at, t=t: e.dma_start(out=a_d[t * 128:(t + 1) * 128, :], in_=at[:]), reads=[at], writes=[a_d])

        if stage < 3:
            P.finish()
            return nc
        ub = P.sb("ub", [128, 512], F32)
        gvb = P.sb("gvb", [128, 512], F32)
        t1 = P.sb("t1", [128, 512], F32)
        vn = P.sb("vn", [128, 512], BF16)
        bsg = P.sbn("bsg", [128, 512], BF16, 2)
        for t in range(18):
            s = 0 if t < 16 else 1
            xt = xbuf[t % 2]
            P.dma('sp', lambda e, xt=xt, t=t: e.dma_start(out=xt[:], in_=xq_d[t * 128:(t + 1) * 128, :]), reads=[xq_d], writes=[xt])
            emit_norm_mod(P, xt, xt[:], gs[s], sh[s], hb[t % 2], hb[t % 2][:], junk, ss[t % 2], rstd[t % 2])
            emit_T(P, C, hb[t % 2], hT[t % 2], psT[t % 2])
            ht = hT[t % 2]
            pu, pg = psS[0], psS[1]
            for c in range(8):
                P.op('pe', lambda e, c=c, ht=ht: e.matmul(pu[:, 0:512], lhsT=ht[:, c, :], rhs=wug[:, c, 0:512], start=(c == 0), stop=(c == 7)), reads=[ht, wug], writes=[pu])
            for c in range(8):
                P.op('pe', lambda e, c=c, ht=ht: e.matmul(pg[:, 0:512], lhsT=ht[:, c, :], rhs=wug[:, c, 512:1024], start=(c == 0), stop=(c == 7)), reads=[ht, wug], writes=[pg])
            emit_gelu(P, pu, ub, t1, 512)
            emit_gelu(P, pg, gvb, t1, 512)
            emit_norm_mod(P, gvb, gvb[:], sgug, None, vn, vn[:], junk, ss[t % 2], rstd[t % 2], D=512)
            pm = psO[t % 2]
            for g in range(8):
                P.op('pe', lambda e, g=g, pm=pm: e.matmul(pm[:, g * 64:(g + 1) * 64], lhsT=swT[:, g, :], rhs=vn[:, g * 64:(g + 1) * 64], start=True, stop=True),
                     reads=[swT, vn], writes=[pm])
            bo = bsg[t % 2]
            for g in range(8):
                P.op('dve', lambda e, g=g, pm=pm, bo=bo: e.scalar_tensor_tensor(out=bo[:, g * 64:(g + 1) * 64], in0=pm[:, g * 64:(g + 1) * 64], scalar=sbT[:, g:g + 1],
                                                                            in1=ub[:, g * 64:(g + 1) * 64], op0=ALU.add, op1=ALU.mult), reads=[pm, sbT, ub], writes=[bo])
            P.dma('sp', lambda e, bo=bo, t=t: e.dma_start(out=bsg_d[t * 128:(t + 1) * 128, :], in_=bo[:]), reads=[bo], writes=[bsg_d])
        P.finish()
    return nc


def build_p2(KIN, ntiles, nlat):
    KC = KIN // 128
    ntok = ntiles * 128
    nc = bass.Bass("TRN2", target_bir_lowering=False)
    with ExitStack() as es:
        P = Prog(nc, es)
        Din = lambda n, s, dt=F32: P.dram(n, s, dt, "ExternalInput")
        x_d = Din("x_tok", [ntok, 1024])
        ab_d = Din("ab", [ntok, KIN], BF16)
        condT_d = Din("condT", [128, 16])
        adaw_d = Din("adaw", [1024, 6144])
        adab_d = Din("adab", [1, 6144])
        n2g_d = Din("n2g", [1, 1024])
        wout_d = Din("wout", [KIN, 1024])
        wr_d = Din("wr", [1024, 16])
        x1_d = P.dram("x1_out", [ntok, 1024], F32, "ExternalOutput")
        h2_d = P.dram("h2_out", [ntok, 1024], BF16, "ExternalOutput")
        affT_d = P.dram("affT_out", [16, ntok], F32, "ExternalOutput")

        C = make_consts(P)
        psS = [P.ps("psS%d" % i, [128, 1024], F32) for i in range(2)]
        psT = [P.ps("psT%d" % i, [128, 8, 128], BF16) for i in range(2)]
        psO = [P.ps("psO%d" % i, [128, 512], F32) for i in range(2)]
        mod = emit_adaln(P, C, condT_d, adaw_d, adab_d, 2048, 3, psO, "a")
        n2g = P.sb("n2g", [128, 1024], F32)
        P.dma('sp', lambda e: e.dma_start(out=n2g[:], in_=n2g_d[0:1, :].to_broadcast([128, 1024])), reads=[n2g_d], writes=[n2g])
        g1 = [mod[0][0], mod[1][0]]
        sh = [mod[0][1], mod[1][1]]
        gs = [mod[0][2], mod[1][2]]
        for s in range(2):
            P.op('dve', lambda e, s=s: e.scalar_tensor_tensor(out=gs[s][:], in0=gs[s][:], scalar=1.0, in1=n2g[:], op0=ALU.add, op1=ALU.mult),
                 reads=[gs[s], n2g], writes=[gs[s]])
        wout = P.sb("wout", [128, KC, 1024], BF16)
        for c0 in range(0, KC, 8):
            P.dma('pool', lambda e, c0=c0: e.dma_start(out=wout[:, c0:c0 + 8, :], in_=wview(wout_d)[:, c0:c0 + 8, :]), reads=[wout_d], writes=[wout])
        wr = P.sb("wr", [128, 8, 16], F32)
        P.dma('sp', lambda e: e.dma_start(out=wr[:], in_=wview(wr_d)), reads=[wr_d], writes=[wr])

        xbuf = P.sbn("xbuf", [128, 1024], F32, 2)
        abt = P.sbn("abt", [128, KIN], BF16, 2)
        abT = P.sbn("abT", [128, KC, 128], BF16, 2)
        tmp = P.sb("tmp", [128, 1024], F32)
        x1 = P.sbn("x1", [128, 1024], F32, 2)
        h2f = P.sbn("h2f", [128, 1024], F32, 2)
        h2b = P.sbn("h2b", [128, 1024], BF16, 2)
        h2T = P.sb("h2T", [128, 8, 128], F32)
        junk = P.sb("junk", [128, 1024], F32)
        ss = P.sbn("ss", [128, 1], F32, 2)
        rstd = P.sbn("rstd", [128, 1], F32, 2)
        lg = P.sb("lg", [128, 16], F32)
        ex = P.sb("ex", [128, 16], F32)
        af = P.sb("af", [128, 16], F32)
        nm = P.sb("nm", [128, 1], F32)
        rs = P.sb("rs", [128, 1], F32)
        affT = P.sb("affT", [16, ntok], F32)

        for t in range(ntiles):
            s = 0 if t < nlat else 1
            xt = xbuf[t % 2]
            at = abt[t % 2]
            aT = abT[t % 2]
            P.dma('sp', lambda e, xt=xt, t=t: e.dma_start(out=xt[:], in_=x_d[t * 128:(t + 1) * 128, :]), reads=[x_d], writes=[xt])
            P.dma('sp', lambda e, at=at, t=t: e.dma_start(out=at[:], in_=ab_d[t * 128:(t + 1) * 128, :]), reads=[ab_d], writes=[at])
            for c0 in range(0, KC, 8):
                pT = psT[(c0 // 8) % 2]
                for c in range(8):
                    P.op('pe', lambda e, c=c, c0=c0, pT=pT, at=at: e.transpose(out=pT[:, c, :], in_=at[:, (c0 + c) * 128:(c0 + c + 1) * 128], identity=C['ident'][:, :]),
                         reads=[at, C['ident']], writes=[pT])
                P.op('act', lambda e, c0=c0, pT=pT, aT=aT: e.copy(out=aT[:, c0:c0 + 8, :], in_=pT[:, :, :]), reads=[pT], writes=[aT])
            x1t = x1[t % 2]
            for half in range(2):
                ps = psS[half]
                for c in range(KC):
                    P.op('pe', lambda e, c=c, ps=ps, aT=aT, half=half: e.matmul(ps[:, 0:512], lhsT=aT[:, c, :], rhs=wout[:, c, half * 512:(half + 1) * 512],
                                                                           start=(c == 0), stop=(c == KC - 1)), reads=[aT, wout], writes=[ps])
                hs = slice(half * 512, half * 512 + 512)
                P.op('dve', lambda e, ps=ps, hs=hs, s=s: e.tensor_tensor(out=tmp[:, hs], in0=ps[:, 0:512], in1=g1[s][:, hs], op=ALU.mult), reads=[ps, g1[s]], writes=[tmp])
                P.op('dve', lambda e, hs=hs, xt=xt, x1t=x1t: e.tensor_tensor(out=x1t[:, hs], in0=tmp[:, hs], in1=xt[:, hs], op=ALU.add), reads=[tmp, xt], writes=[x1t])
            P.dma('sp', lambda e, x1t=x1t, t=t: e.dma_start(out=x1_d[t * 128:(t + 1) * 128, :], in_=x1t[:]), reads=[x1t], writes=[x1_d])
            hf = h2f[t % 2]
            hbt = h2b[t % 2]
            emit_norm_mod(P, x1t, x1t[:], gs[s], sh[s], hf, hf[:], junk, ss[t % 2], rstd[t % 2])
            P.op('act', lambda e, hf=hf, hbt=hbt: e.copy(out=hbt[:], in_=hf[:]), reads=[hf], writes=[hbt])
            P.dma('sp', lambda e, hbt=hbt, t=t: e.dma_start(out=h2_d[t * 128:(t + 1) * 128, :], in_=hbt[:]), reads=[hbt], writes=[h2_d])
            pS = psS[t % 2]
            for c in range(8):
                P.op('pe', lambda e, c=c, pS=pS, hf=hf: e.transpose(out=pS[:, c * 128:(c + 1) * 128], in_=hf[:, c * 128:(c + 1) * 128], identity=C['identf'][:, :]),
                     reads=[hf, C['identf']], writes=[pS])
            P.op('act', lambda e, pS=pS: e.copy(out=h2T[:, :, :], in_=pS[:, :].rearrange("p (c n) -> p c n", c=8)), reads=[pS], writes=[h2T])
            po = psO[t % 2]
            for c in range(8):
                P.op('pe', lambda e, c=c, po=po: e.matmul(po[:, 0:16], lhsT=h2T[:, c, :], rhs=wr[:, c, :], start=(c == 0), stop=(c == 7)), reads=[h2T, wr], writes=[po])
            P.op('dve', lambda e, po=po: e.tensor_reduce(out=nm[:], in_=po[:, 0:16], axis=AX.X, op=ALU.max, negate=True), reads=[po], writes=[nm])
            P.op('act', lambda e, po=po: e.activation(out=ex[:], in_=po[:, 0:16], func=AF.Exp, bias=nm[:, 0:1], scale=1.0, accum_out=rs[:]), reads=[po, nm], writes=[ex, rs])
            P.op('dve', lambda e: e.reciprocal(out=rs[:], in_=rs[:]), reads=[rs], writes=[rs])
            P.op('dve', lambda e: e.tensor_scalar(out=af[:], in0=ex[:], scalar1=rs[:, 0:1], scalar2=None, op0=ALU.mult), reads=[ex, rs], writes=[af])
            P.op('pe', lambda e, po=po: e.transpose(out=po[0:16, 128:256], in_=af[:, 0:16], identity=C['identf'][:, :]), reads=[af, C['identf']], writes=[po])
            P.op('act', lambda e, po=po, t=t: e.copy(out=affT[:, t * 128:(t + 1) * 128], in_=po[0:16, 128:256]), reads=[po], writes=[affT])
        P.dma('sp', lambda e: e.dma_start(out=affT_d[:, :], in_=affT[:]), reads=[affT], writes=[affT_d])
        P.finish()
    return nc


def build_p3(has_ctx, nbis=30):
    NJ = 66 if has_ctx else 64
    NTB = NJ * 128
    NS = 1056 if has_ctx else 1024
    nc = bass.Bass("TRN2", target_bir_lowering=False)
    with ExitStack() as es:
        P = Prog(nc, es)
        Din = lambda n, s, dt=F32: P.dram(n, s, dt, "ExternalInput")
        aff4_d = Din("aff4", [4, NTB])
        h2_d = Din("h2_all", [2, NTB, 1024], BF16)
        wg_d = Din("wg", [2, 1024, 2816])
        wu_d = Din("wu", [2, 1024, 2816])
        wd_d = Din("wd", [2, 2816, 1024])
        G_d = Din("Gc", [128, 128])
        Tri_d = Din("Tric", [128, 128])
        ye_d = P.dram("ye_out", [4, NS, 1024], F32, "ExternalOutput")
        dest_d = P.dram("dest_out", [128, 4 * NJ], F32, "ExternalOutput")
        xe_d = P.dram("xe_scr", [4 * NS, 1024], BF16, "Internal")
        thr_d = P.dram("thr_scr", [128, 2], F32, "Internal")
        xe_d.acc = True

        C = make_consts(P)
        psS = [P.ps("psS%d" % i, [128, 1024], F32) for i in range(2)]
        psT = [P.ps("psT%d" % i, [128, 8, 128], BF16) for i in range(2)]
        psO = [P.ps("psO%d" % i, [128, 512], F32) for i in range(2)]

        G = P.sb("G", [128, 128], F32)
        Tri = P.sb("Tri", [128, 128], BF16)
        onesb = P.sb("onesb", [128, 128], BF16)
        P.dma('sp', lambda e: e.dma_start(out=G[:], in_=G_d[:, :]), reads=[G_d], writes=[G])
        P.dma('pool', lambda e: e.dma_start(out=Tri[:], in_=Tri_d[:, :]), reads=[Tri_d], writes=[Tri])
        P.op('dve', lambda e: e.tensor_copy(out=onesb[:], in_=C['ones'][:]), reads=[C['ones']], writes=[onesb])

        affR = P.sb("affR", [128, 256], F32)
        affRc = P.sb("affRc", [128, 8], F32)
        for be in range(4):
            P.dma('sp', lambda e, be=be: e.dma_start(out=affR[be * 32:(be + 1) * 32, :], in_=aff4_d[be:be + 1, 0:8192].rearrange("o (s c) -> (o s) c", c=256)),
                  reads=[aff4_d], writes=[affR])
            if has_ctx:
                P.dma('sp', lambda e, be=be: e.dma_start(out=affRc[be * 32:(be + 1) * 32, :], in_=aff4_d[be:be + 1, 8192:8448].rearrange("o (s c) -> (o s) c", c=8)),
                      reads=[aff4_d], writes=[affRc])
        lo2 = P.sb("lo2", [128, 2], F32)
        hi2 = P.sb("hi2", [128, 2], F32)
        mid2 = P.sb("mid2", [128, 2], F32)
        cnt2 = P.sb("cnt2", [128, 2], F32)
        cap2 = P.sb("cap2", [128, 2], F32)
        sel = P.sb("sel", [128, 2], I32)
        nsel = P.sb("nsel", [128, 2], I32)
        bj = P.sb("bj", [128, 256], F32)
        P.op('dve', lambda e: e.memset(lo2[:], 0.0), writes=[lo2])
        P.op('dve', lambda e: e.memset(hi2[:], 1.0), writes=[hi2])
        P.op('dve', lambda e: e.memset(cnt2[:], 0.0), writes=[cnt2])
        P.op('dve', lambda e: e.memset(cap2[:, 0:1], 1024.0), writes=[cap2])
        P.op('dve', lambda e: e.memset(cap2[:, 1:2], 32.0), writes=[cap2])
        pt = psO[0]
        for it in range(nbis):
            P.op('dve', lambda e: e.tensor_tensor(out=mid2[:], in0=lo2[:], in1=hi2[:], op=ALU.add), reads=[lo2, hi2], writes=[mid2])
            P.op('dve', lambda e: e.tensor_scalar(out=mid2[:], in0=mid2[:], scalar1=0.5, scalar2=None, op0=ALU.mult), reads=[mid2], writes=[mid2])
            P.op('dve', lambda e: e.tensor_scalar(out=bj[:, 0:256], in0=affR[:], scalar1=mid2[:, 0:1], scalar2=None, op0=ALU.is_ge, op1=ALU.add, accum_out=cnt2[:, 0:1]),
                 reads=[affR, mid2], writes=[bj, cnt2])
            if has_ctx:
                P.op('dve', lambda e: e.tensor_scalar(out=bj[:, 0:8], in0=affRc[:], scalar1=mid2[:, 1:2], scalar2=None, op0=ALU.is_ge, op1=ALU.add, accum_out=cnt2[:, 1:2]),
                     reads=[affRc, mid2], writes=[bj, cnt2])
            P.op('pe', lambda e: e.matmul(pt[:, 0:2], lhsT=G[:, :], rhs=cnt2[:, :], start=True, stop=True), reads=[G, cnt2], writes=[pt])
            P.op('dve', lambda e: e.tensor_tensor(out=sel[:], in0=pt[:, 0:2], in1=cap2[:], op=ALU.is_ge), reads=[pt, cap2], writes=[sel])
            P.op('dve', lambda e: e.tensor_tensor(out=nsel[:], in0=pt[:, 0:2], in1=cap2[:], op=ALU.is_lt), reads=[pt, cap2], writes=[nsel])
            P.op('dve', lambda e: e.copy_predicated(out=lo2[:], mask=sel[:], data=mid2[:]), reads=[sel, mid2], writes=[lo2])
            P.op('dve', lambda e: e.copy_predicated(out=hi2[:], mask=nsel[:], data=mid2[:]), reads=[nsel, mid2], writes=[hi2])
        P.dma('sp', lambda e: e.dma_start(out=thr_d[:, :], in_=lo2[:]), reads=[lo2], writes=[thr_d])
        thr_bc = P.sb("thr_bc", [128, 4, 2], F32)
        P.dma('sp', lambda e: e.dma_start(out=thr_bc[:], in_=thr_d.t.rearrange("(b s) c -> b (s c)", s=32)[:, 0:2].partition_broadcast(128)),
              reads=[thr_d], writes=[thr_bc])

        A4 = P.sb("A4", [4, NTB], F32)
        P.dma('sp', lambda e: e.dma_start(out=A4[:], in_=aff4_d[:, :]), reads=[aff4_d], writes=[A4])
        pA = psO[1]
        for j in range(NJ):
            P.op('pe', lambda e, j=j: e.transpose(out=pA[:, j * 4:(j + 1) * 4], in_=A4[0:4, j * 128:(j + 1) * 128], identity=C['identf'][0:4, 0:4]),
                 reads=[A4, C['identf']], writes=[pA])
        aff_tm = P.sb("aff_tm", [128, 4, NJ], F32)
        P.op('dve', lambda e: e.tensor_copy(out=aff_tm[:], in_=pA[:, 0:4 * NJ].rearrange("p (j b) -> p b j", b=4)), reads=[pA], writes=[aff_tm])
        mask = P.sb("mask", [128, 4, NJ], F32)
        maskb = P.sb("maskb", [128, 4, NJ], BF16)
        P.op('dve', lambda e: e.tensor_tensor(out=mask[:, :, 0:64], in0=aff_tm[:, :, 0:64], in1=thr_bc[:, :, 0:1].to_broadcast([128, 4, 64]), op=ALU.is_ge),
             reads=[aff_tm, thr_bc], writes=[mask])
        if has_ctx:
            P.op('dve', lambda e: e.tensor_tensor(out=mask[:, :, 64:66], in0=aff_tm[:, :, 64:66], in1=thr_bc[:, :, 1:2].to_broadcast([128, 4, 2]), op=ALU.is_ge),
                 reads=[aff_tm, thr_bc], writes=[mask])
        P.op('dve', lambda e: e.tensor_copy(out=maskb[:], in_=mask[:]), reads=[mask], writes=[maskb])
        pW = psS[0]
        pTt = psS[1]
        mflat = maskb[:].rearrange("p b j -> p (b j)")
        P.op('pe', lambda e: e.matmul(pW[:, 0:4 * NJ], lhsT=Tri[:, :], rhs=mflat, start=True, stop=True), reads=[Tri, maskb], writes=[pW])
        P.op('pe', lambda e: e.matmul(pTt[:, 0:4 * NJ], lhsT=onesb[:, :], rhs=mflat, start=True, stop=True), reads=[onesb, maskb], writes=[pTt])
        tot = P.sb("tot", [128, 4, NJ], F32)
        incl = P.sb("incl", [128, 4, NJ], F32)
        dest = P.sb("dest", [128, 4, NJ], F32)
        vl = P.sb("vl", [128, 4, NJ], F32)
        base = P.sb("base", [128, 4, NJ], F32)
        dest_i = P.sb("dest_i", [128, 4 * NJ], I32)
        onesr = P.sb("onesr", [128, 64], F32)
        P.op('dve', lambda e: e.memset(onesr[:], 1.0), writes=[onesr])
        P.op('dve', lambda e: e.tensor_copy(out=tot[:], in_=pTt[:, 0:4 * NJ].rearrange("p (b j) -> p b j", b=4)), reads=[pTt], writes=[tot])
        for be in range(4):
            P.op('dve', lambda e, be=be: e.tensor_tensor_scan(out=incl[:, be, 0:64], data0=onesr[:, 0:64], data1=tot[:, be, 0:64], initial=0.0, op0=ALU.mult, op1=ALU.add),
                 reads=[onesr, tot], writes=[incl])
            P.op('dve', lambda e, be=be: e.memset(base[:, be, 0:64], float(be * NS)), writes=[base])
            if has_ctx:
                P.op('dve', lambda e, be=be: e.memset(base[:, be, 64:66], float(be * NS + 1024)), writes=[base])
        if has_ctx:
            P.op('dve', lambda e: e.tensor_copy(out=incl[:, :, 64:65], in_=tot[:, :, 64:65]), reads=[tot], writes=[incl])
            P.op('dve', lambda e: e.tensor_tensor(out=incl[:, :, 65:66], in0=tot[:, :, 64:65], in1=tot[:, :, 65:66], op=ALU.add), reads=[tot], writes=[incl])
        P.op('dve', lambda e: e.tensor_tensor(out=incl[:], in0=incl[:], in1=tot[:], op=ALU.subtract), reads=[incl, tot], writes=[incl])
        P.op('dve', lambda e: e.tensor_tensor(out=dest[:], in0=pW[:, 0:4 * NJ].rearrange("p (b j) -> p b j", b=4), in1=incl[:], op=ALU.add), reads=[pW, incl], writes=[dest])
        P.op('dve', lambda e: e.tensor_scalar(out=vl[:, :, 0:64], in0=dest[:, :, 0:64], scalar1=1024.0, scalar2=None, op0=ALU.is_lt), reads=[dest], writes=[vl])
        if has_ctx:
            P.op('dve', lambda e: e.tensor_scalar(out=vl[:, :, 64:66], in0=dest[:, :, 64:66], scalar1=32.0, scalar2=None, op0=ALU.is_lt), reads=[dest], writes=[vl])
        P.op('dve', lambda e: e.tensor_tensor(out=vl[:], in0=vl[:], in1=mask[:], op=ALU.mult), reads=[vl, mask], writes=[vl])
        P.op('dve', lambda e: e.tensor_tensor(out=dest[:], in0=dest[:], in1=base[:], op=ALU.add), reads=[dest, base], writes=[dest])
        P.op('dve', lambda e: e.tensor_scalar(out=vl[:], in0=vl[:], scalar1=-1.0, scalar2=-BIG, op0=ALU.add, op1=ALU.mult), reads=[vl], writes=[vl])
        P.op('dve', lambda e: e.tensor_tensor(out=dest[:], in0=dest[:], in1=vl[:], op=ALU.add), reads=[dest, vl], writes=[dest])
        P.op('dve', lambda e: e.tensor_copy(out=dest_i[:], in_=dest[:].rearrange("p b j -> p (b j)")), reads=[dest], writes=[dest_i])
        P.dma('sp', lambda e: e.dma_start(out=dest_d[:, :], in_=dest[:].rearrange("p b j -> p (b j)")), reads=[dest], writes=[dest_d])

        hbuf = P.sbn("hbuf", [128, 1024], BF16, 2)
        k = 0
        for b in range(2):
            for j in range(NJ):
                hb_ = hbuf[k % 2]
                k += 1
                P.dma('sp', lambda e, hb_=hb_, b=b, j=j: e.dma_start(out=hb_[:], in_=h2_d[b, j * 128:(j + 1) * 128, :]), reads=[h2_d], writes=[hb_])
                for el in range(2):
                    be = b * 2 + el
                    P.dma('pool', lambda e, hb_=hb_, be=be, j=j: e.indirect_dma_start(
                        out=xe_d[:, :], out_offset=bass.IndirectOffsetOnAxis(ap=dest_i[:, be * NJ + j:be * NJ + j + 1], axis=0),
                        in_=hb_[:, :], in_offset=None, bounds_check=P.reg(e, 4 * NS - 1), oob_is_err=False),
                        reads=[hb_, dest_i], writes=[xe_d])

        wd_sb = P.sb("wd_sb", [128, 22, 1024], BF16)
        xeT = P.sb("xeT", [128, 8, NS], BF16)
        hidT = P.sb("hidT", [128, 22, NS], BF16)
        wgb = P.sbn("wgb", [128, 8, 256], BF16, 2)
        wub = P.sbn("wub", [128, 8, 256], BF16, 2)
        xt = P.sbn("xt", [128, 1024], BF16, 2)
        tmp = P.sbn("tmpf", [128, 512], F32, 2)
        yet = P.sbn("yet", [128, 1024], F32, 2)
        stiles = [(st * 128, 128) for st in range(8)] + ([(1024, 32)] if has_ctx else [])
        sgroups = [(0, 512), (512, 512)] + ([(1024, 32)] if has_ctx else [])
        wi = 0
        gi = 0
        for el in range(2):
            for c0 in range(0, 22, 6):
                c1 = min(22, c0 + 6)
                P.dma('pool', lambda e, el=el, c0=c0, c1=c1: e.dma_start(out=wd_sb[:, c0:c1, :], in_=wd_d.t[el].rearrange("(c p) d -> p c d", p=128)[:, c0:c1, :]),
                      reads=[wd_d], writes=[wd_sb])
            for b in range(2):
                be = b * 2 + el
                for si, (s0, rows) in enumerate(stiles):
                    x_ = xt[si % 2]
                    P.dma('sp', lambda e, x_=x_, be=be, s0=s0, rows=rows: e.dma_start(out=x_[0:rows, :], in_=xe_d[be * NS + s0:be * NS + s0 + rows, :]), reads=[xe_d], writes=[x_])
                    pT = psT[si % 2]
                    for c in range(8):
                        P.op('pe', lambda e, c=c, pT=pT, x_=x_, rows=rows: e.transpose(out=pT[:, c, 0:rows], in_=x_[0:rows, c * 128:(c + 1) * 128], identity=C['ident'][0:rows, 0:rows]),
                             reads=[x_, C['ident']], writes=[pT])
                    P.op('act', lambda e, pT=pT, s0=s0, rows=rows: e.copy(out=xeT[:, :, s0:s0 + rows], in_=pT[:, :, 0:rows]), reads=[pT], writes=[xeT])
                for fg in range(11):
                    wg_ = wgb[wi % 2]
                    wu_ = wub[wi % 2]
                    wi += 1
                    P.dma('pool', lambda e, wg_=wg_, el=el, fg=fg: e.dma_start(out=wg_[:], in_=wg_d.t[el].rearrange("(c p) f -> p c f", p=128)[:, :, fg * 256:(fg + 1) * 256]),
                          reads=[wg_d], writes=[wg_])
                    P.dma('pool', lambda e, wu_=wu_, el=el, fg=fg: e.dma_start(out=wu_[:], in_=wu_d.t[el].rearrange("(c p) f -> p c f", p=128)[:, :, fg * 256:(fg + 1) * 256]),
                          reads=[wu_d], writes=[wu_])
                    for fc in range(2):
                        fi = fg * 2 + fc
                        for (s0, n) in sgroups:
                            pS = psS[gi % 2]
                            tm = tmp[gi % 2]
                            gi += 1
                            for c in range(8):
                                P.op('pe', lambda e, c=c, pS=pS, wg_=wg_, fc=fc, s0=s0, n=n: e.matmul(pS[:, 0:n], lhsT=wg_[:, c, fc * 128:(fc + 1) * 128], rhs=xeT[:, c, s0:s0 + n],
                                                                                                  start=(c == 0), stop=(c == 7)), reads=[wg_, xeT], writes=[pS])
                            for c in range(8):
                                P.op('pe', lambda e, c=c, pS=pS, wu_=wu_, fc=fc, s0=s0, n=n: e.matmul(pS[:, 512:512 + n], lhsT=wu_[:, c, fc * 128:(fc + 1) * 128], rhs=xeT[:, c, s0:s0 + n],
                                                                                                  start=(c == 0), stop=(c == 7)), reads=[wu_, xeT], writes=[pS])
                            P.op('act', lambda e, pS=pS, tm=tm, n=n: e.activation(out=tm[:, 0:n], in_=pS[:, 0:n], func=AF.Silu), reads=[pS], writes=[tm])
                            P.op('dve', lambda e, pS=pS, tm=tm, n=n, fi=fi, s0=s0: e.tensor_tensor(out=hidT[:, fi, s0:s0 + n], in0=tm[:, 0:n], in1=pS[:, 512:512 + n], op=ALU.mult),
                                 reads=[tm, pS], writes=[hidT])
                for si, (s0, rows) in enumerate(stiles):
                    y_ = yet[si % 2]
                    for dh in range(2):
                        pD = psO[dh]
                        for fi in range(22):
                            P.op('pe', lambda e, fi=fi, pD=pD, s0=s0, rows=rows, dh=dh: e.matmul(pD[0:rows, 0:512], lhsT=hidT[:, fi, s0:s0 + rows], rhs=wd_sb[:, fi, dh * 512:(dh + 1) * 512],
                                                                                             start=(fi == 0), stop=(fi == 21)), reads=[hidT, wd_sb], writes=[pD])
                        if dh == 0:
                            P.op('act', lambda e, pD=pD, y_=y_, rows=rows: e.copy(out=y_[0:rows, 0:512], in_=pD[0:rows, 0:512]), reads=[pD], writes=[y_])
                        else:
                            P.op('dve', lambda e, pD=pD, y_=y_, rows=rows: e.tensor_copy(out=y_[0:rows, 512:1024], in_=pD[0:rows, 0:512]), reads=[pD], writes=[y_])
                    P.dma('sp', lambda e, y_=y_, be=be, s0=s0, rows=rows: e.dma_start(out=ye_d[be, s0:s0 + rows, :], in_=y_[0:rows, :]), reads=[y_], writes=[ye_d])
        P.finish()
    return nc


def build_p4(ntiles, nlat, NS, last):
    ntok = ntiles * 128
    nc = bass.Bass("TRN2", target_bir_lowering=False)
    with ExitStack() as es:
        P = Prog(nc, es)
        Din = lambda n, s, dt=F32: P.dram(n, s, dt, "ExternalInput")
        x1_d = Din("x1", [ntok, 1024])
        affT_d = Din("affT", [16, ntok])
        dest_d = Din("dest16", [128, ntiles * 16])
        offs_d = Din("offs", [128, 16])
        ye_d = Din("ye_b", [16 * NS, 1024])
        condT_d = Din("condT", [128, 16])
        adaw_d = Din("adaw", [1024, 6144])
        adab_d = Din("adab", [1, 6144])
        if last:
            fg_d = Din("fng", [1, 1024])
            out_d = P.dram("y_out", [ntok, 1024], F32, "ExternalOutput")
        else:
            adawn_d = Din("adaw_n", [1024, 6144])
            adabn_d = Din("adab_n", [1, 6144])
            n1g_d = Din("n1g_n", [1, 1024])
            x2_d = P.dram("x2_out", [ntok, 1024], F32, "ExternalOutput")
            hn_d = P.dram("hn_out", [ntok, 1024], BF16, "ExternalOutput")

        C = make_consts(P)
        psO = [P.ps("psO%d" % i, [128, 512], F32) for i in range(2)]
        mod = emit_adaln(P, C, condT_d, adaw_d, adab_d, 5120, 1, psO, "a")
        g2 = [mod[0][0], mod[1][0]]
        ng = P.sb("ng", [128, 1024], F32)
        if last:
            P.dma('sp', lambda e: e.dma_start(out=ng[:], in_=fg_d[0:1, :].to_broadcast([128, 1024])), reads=[fg_d], writes=[ng])
        else:
            modn = emit_adaln(P, C, condT_d, adawn_d, adabn_d, 0, 2, psO, "n")
            P.dma('sp', lambda e: e.dma_start(out=ng[:], in_=n1g_d[0:1, :].to_broadcast([128, 1024])), reads=[n1g_d], writes=[ng])
            shn = [modn[0][0], modn[1][0]]
            gsn = [modn[0][1], modn[1][1]]
            for s in range(2):
                P.op('dve', lambda e, s=s: e.scalar_tensor_tensor(out=gsn[s][:], in0=gsn[s][:], scalar=1.0, in1=ng[:], op0=ALU.add, op1=ALU.mult),
                     reads=[gsn[s], ng], writes=[gsn[s]])
        affT = P.sb("affT", [16, ntok], F32)
        P.dma('sp', lambda e: e.dma_start(out=affT[:], in_=affT_d[:, :]), reads=[affT_d], writes=[affT])
        dest = P.sb("dest", [128, ntiles * 16], F32)
        P.dma('sp', lambda e: e.dma_start(out=dest[:], in_=dest_d[:, :]), reads=[dest_d], writes=[dest])
        offs = P.sb("offs", [128, 16], F32)
        P.dma('sp', lambda e: e.dma_start(out=offs[:], in_=offs_d[:, :]), reads=[offs_d], writes=[offs])
        NB = 4
        gt = P.sbn("gt", [128, 1024], F32, NB)
        for i in range(NB):
            P.op('dve', lambda e, i=i: e.memset(gt[i][:], 0.0), writes=[gt[i]])
        xbuf = P.sbn("xbuf", [128, 1024], F32, 2)
        acc = P.sbn("acc", [128, 1024], F32, 2)
        x2 = P.sbn("x2", [128, 1024], F32, 2)
        hn = P.sbn("hn", [128, 1024], BF16, 2)
        yo = P.sbn("yo", [128, 1024], F32, 2)
        junk = P.sb("junk", [128, 1024], F32)
        ss = P.sbn("ss", [128, 1], F32, 2)
        rstd = P.sbn("rstd", [128, 1], F32, 2)
        gate = P.sbn("gate", [128, 16], F32, 2)
        vl = P.sbn("vl", [128, 16], F32, 2)
        dd = P.sbn("dd", [128, 16], F32, 2)
        di = P.sbn("di", [128, 16], I32, 2)
        gi = 0
        for t in range(ntiles):
            s = 0 if t < nlat else 1
            xt = xbuf[t % 2]
            P.dma('sp', lambda e, xt=xt, t=t: e.dma_start(out=xt[:], in_=x1_d[t * 128:(t + 1) * 128, :]), reads=[x1_d], writes=[xt])
            po = psO[t % 2]
            P.op('pe', lambda e, po=po, t=t: e.transpose(out=po[:, 0:16], in_=affT[0:16, t * 128:(t + 1) * 128], identity=C['identf'][0:16, 0:16]),
                 reads=[affT, C['identf']], writes=[po])
            ga, v_, d_, di_ = gate[t % 2], vl[t % 2], dd[t % 2], di[t % 2]
            dsl = dest[:, t * 16:(t + 1) * 16]
            P.op('dve', lambda e, v_=v_, dsl=dsl: e.tensor_scalar(out=v_[:], in0=dsl, scalar1=1.0e5, scalar2=None, op0=ALU.is_lt), reads=[dest], writes=[v_])
            P.op('dve', lambda e, ga=ga, po=po, v_=v_: e.tensor_tensor(out=ga[:], in0=po[:, 0:16], in1=v_[:], op=ALU.mult), reads=[po, v_], writes=[ga])
            P.op('dve', lambda e, d_=d_, dsl=dsl: e.tensor_tensor(out=d_[:], in0=dsl, in1=offs[:], op=ALU.add), reads=[dest, offs], writes=[d_])
            P.op('dve', lambda e, d_=d_, di_=di_: e.tensor_copy(out=di_[:], in_=d_[:]), reads=[d_], writes=[di_])
            ac = acc[t % 2]
            for ex in range(16):
                g_ = gt[gi % NB]
                gi += 1
                P.dma('pool', lambda e, g_=g_, di_=di_, ex=ex: e.indirect_dma_start(
                    out=g_[:, :], out_offset=None, in_=ye_d[:, :], in_offset=bass.IndirectOffsetOnAxis(ap=di_[:, ex:ex + 1], axis=0),
                    bounds_check=P.reg(e, 16 * NS - 1), oob_is_err=False), reads=[ye_d, di_], writes=[g_])
                if ex == 0:
                    P.op('dve', lambda e, g_=g_, ac=ac, ga=ga: e.tensor_scalar(out=ac[:], in0=g_[:], scalar1=ga[:, 0:1], scalar2=None, op0=ALU.mult), reads=[g_, ga], writes=[ac])
                else:
                    P.op('dve', lambda e, g_=g_, ac=ac, ga=ga, ex=ex: e.scalar_tensor_tensor(out=ac[:], in0=g_[:], scalar=ga[:, ex:ex + 1], in1=ac[:], op0=ALU.mult, op1=ALU.add),
                         reads=[g_, ga, ac], writes=[ac])
            x2t = x2[t % 2]
            P.op('dve', lambda e, ac=ac, s=s: e.tensor_tensor(out=ac[:], in0=ac[:], in1=g2[s][:], op=ALU.mult), reads=[ac, g2[s]], writes=[ac])
            P.op('dve', lambda e, ac=ac, xt=xt, x2t=x2t: e.tensor_tensor(out=x2t[:], in0=ac[:], in1=xt[:], op=ALU.add), reads=[ac, xt], writes=[x2t])
            if last:
                y_ = yo[t % 2]
                emit_norm_mod(P, x2t, x2t[:], ng, None, y_, y_[:], junk, ss[t % 2], rstd[t % 2])
                P.dma('sp', lambda e, y_=y_, t=t: e.dma_start(out=out_d[t * 128:(t + 1) * 128, :], in_=y_[:]), reads=[y_], writes=[out_d])
            else:
                P.dma('sp', lambda e, x2t=x2t, t=t: e.dma_start(out=x2_d[t * 128:(t + 1) * 128, :], in_=x2t[:]), reads=[x2t], writes=[x2_d])
                h_ = hn[t % 2]
                emit_norm_mod(P, x2t, x2t[:], gsn[s], shn[s], h_, h_[:], junk, ss[t % 2], rstd[t % 2])
                P.dma('sp', lambda e, h_=h_, t=t: e.dma_start(out=hn_d[t * 128:(t + 1) * 128, :], in_=h_[:]), reads=[h_], writes=[hn_d])
        P.finish()
    return nc


def build_p5(nlat_tiles=64):
    NL = nlat_tiles
    NTT = NL + 2
    nc = bass.Bass("TRN2", target_bir_lowering=False)
    with ExitStack() as es:
        P = Prog(nc, es)
        Din = lambda n, s, dt=F32: P.dram(n, s, dt, "ExternalInput")
        hl_d = Din("hl", [NTT * 128, 1024], BF16)
        wret_d = Din("wret", [1024, 1536])
        cos_d = Din("cosT", [128, NL * 128])
        sin_d = Din("sinT", [128, NL * 128])
        dlog_d = Din("dlog", [128, 2])
        iota_d = Din("iota", [128, 256])
        msk_d = Din("msk", [128, 256])
        og_d = P.dram("og_out", [NL * 128, 512], BF16, "ExternalOutput")
        of_d = P.dram("of_scr", [NL * 128, 512], F32, "Internal")
        sg_d = P.dram("sg_scr", [NL * 128, 512], BF16, "Internal")

        C = make_consts(P)
        pb = [P.ps("pb%d" % i, [128, 512], F32) for i in range(7)]
        psT = P.ps("psT", [128, 8, 128], BF16)

        wret = P.sb("wret", [128, 8, 1536], BF16)
        for c0 in range(0, 8, 4):
            P.dma('pool', lambda e, c0=c0: e.dma_start(out=wret[:, c0:c0 + 4, :], in_=wview(wret_d)[:, c0:c0 + 4, :]), reads=[wret_d], writes=[wret])
        iota = P.sb("iota", [128, 256], F32)
        msk = P.sb("msk", [128, 256], F32)
        dlog = P.sb("dlog", [128, 2], F32)
        P.dma('sp', lambda e: e.dma_start(out=iota[:], in_=iota_d[:, :]), reads=[iota_d], writes=[iota])
        P.dma('sp', lambda e: e.dma_start(out=msk[:], in_=msk_d[:, :]), reads=[msk_d], writes=[msk])
        P.dma('sp', lambda e: e.dma_start(out=dlog[:], in_=dlog_d[:, :]), reads=[dlog_d], writes=[dlog])
        lg = P.sb("lg", [128, 2], F32)
        nlg = P.sb("nlg", [128, 2], F32)
        c128 = P.sb("c128", [128, 2], F32)
        P.op('act', lambda e: e.activation(out=nlg[:], in_=dlog[:], func=AF.Exp, scale=-1.0), reads=[dlog], writes=[nlg])
        P.op('dve', lambda e: e.tensor_scalar(out=nlg[:], in0=nlg[:], scalar1=1.0, scalar2=None, op0=ALU.add), reads=[nlg], writes=[nlg])
        P.op('act', lambda e: e.activation(out=nlg[:], in_=nlg[:], func=AF.Ln), reads=[nlg], writes=[nlg])
        P.op('dve', lambda e: e.tensor_scalar(out=lg[:], in0=nlg[:], scalar1=-1.0, scalar2=None, op0=ALU.mult), reads=[nlg], writes=[lg])
        P.op('act', lambda e: e.activation(out=c128[:], in_=lg[:], func=AF.Exp, scale=128.0), reads=[lg], writes=[c128])
        qdec = P.sbn("qdec", [128, 128], F32, 2)
        kdec = P.sbn("kdec", [128, 128], F32, 2)
        for d in range(2):
            src = iota[:, 0:128] if d == 0 else iota[:, 128:256]
            P.op('act', lambda e, d=d, src=src: e.activation(out=qdec[d][:], in_=src, func=AF.Exp, scale=lg[:, d:d + 1]), reads=[iota, lg], writes=[qdec[d]])
            P.op('act', lambda e, d=d, src=src: e.activation(out=kdec[d][:], in_=src, func=AF.Exp, scale=nlg[:, d:d + 1]), reads=[iota, nlg], writes=[kdec[d]])

        qT_all = P.sb("qT_all", [128, 2, NL * 128], BF16)
        kT_all = P.sb("kT_all", [128, 2, NTT * 128], BF16)
        v_all = P.sb("v_all", [128, NTT, 512], BF16)
        hlb = P.sbn("hlb", [128, 1024], BF16, 2)
        hT = P.sbn("hT", [128, 8, 128], BF16, 2)
        cst = P.sbn("cst", [128, 128], F32, 2)
        snt = P.sbn("snt", [128, 128], F32, 2)
        rt = [P.sb("rt%d" % i, [128, 128], F32) for i in range(4)]
        sgb = P.sbn("sgb", [128, 512], BF16, 2)

        for t in range(NTT):
            lat = t < NL
            h_ = hlb[t % 2]
            P.dma('sp', lambda e, h_=h_, t=t: e.dma_start(out=h_[:], in_=hl_d[t * 128:(t + 1) * 128, :]), reads=[hl_d], writes=[h_])
            ht = hT[t % 2]
            emit_T(P, C, h_, ht, psT)
            if lat:
                cs, sn = cst[t % 2], snt[t % 2]
                P.dma('sp', lambda e, cs=cs, t=t: e.dma_start(out=cs[:], in_=cos_d[:, t * 128:(t + 1) * 128]), reads=[cos_d], writes=[cs])
                P.dma('sp', lambda e, sn=sn, t=t: e.dma_start(out=sn[:], in_=sin_d[:, t * 128:(t + 1) * 128]), reads=[sin_d], writes=[sn])
            for (name, col0, dstT, scale, bank0) in (("k", 256, kT_all, 0.0625, 0), ("q", 0, qT_all, 1.0, 2)):
                if name == "q" and not lat:
                    continue
                for half in range(2):
                    pp = pb[bank0 + half]
                    for c in range(8):
                        P.op('pe', lambda e, c=c, pp=pp, ht=ht, col0=col0, half=half: e.matmul(pp[:, 0:128], lhsT=wret[:, c, col0 + half * 128:col0 + (half + 1) * 128], rhs=ht[:, c, :],
                                                                                             start=(c == 0), stop=(c == 7)), reads=[wret, ht], writes=[pp])
                p1, p2 = pb[bank0], pb[bank0 + 1]
                sl = slice(t * 128, (t + 1) * 128)
                if lat:
                    P.op('dve', lambda e, p1=p1, cs=cs, scale=scale: e.scalar_tensor_tensor(out=rt[0][:], in0=p1[:, 0:128], scalar=scale, in1=cs[:], op0=ALU.mult, op1=ALU.mult), reads=[p1, cs], writes=[rt[0]])
                    P.op('dve', lambda e, p2=p2, sn=sn, scale=scale: e.scalar_tensor_tensor(out=rt[1][:], in0=p2[:, 0:128], scalar=scale, in1=sn[:], op0=ALU.mult, op1=ALU.mult), reads=[p2, sn], writes=[rt[1]])
                    P.op('dve', lambda e, dstT=dstT, sl=sl: e.tensor_tensor(out=dstT[:, 0, sl], in0=rt[0][:], in1=rt[1][:], op=ALU.subtract), reads=[rt[0], rt[1]], writes=[dstT])
                    P.op('dve', lambda e, p1=p1, sn=sn, scale=scale: e.scalar_tensor_tensor(out=rt[2][:], in0=p1[:, 0:128], scalar=scale, in1=sn[:], op0=ALU.mult, op1=ALU.mult), reads=[p1, sn], writes=[rt[2]])
                    P.op('dve', lambda e, p2=p2, cs=cs, scale=scale: e.scalar_tensor_tensor(out=rt[3][:], in0=p2[:, 0:128], scalar=scale, in1=cs[:], op0=ALU.mult, op1=ALU.mult), reads=[p2, cs], writes=[rt[3]])
                    P.op('dve', lambda e, dstT=dstT, sl=sl: e.tensor_tensor(out=dstT[:, 1, sl], in0=rt[2][:], in1=rt[3][:], op=ALU.add), reads=[rt[2], rt[3]], writes=[dstT])
                else:
                    P.op('act', lambda e, p1=p1, dstT=dstT, sl=sl, scale=scale: e.activation(out=dstT[:, 0, sl], in_=p1[:, 0:128], func=AF.Copy, scale=scale), reads=[p1], writes=[dstT])
                    P.op('act', lambda e, p2=p2, dstT=dstT, sl=sl, scale=scale: e.activation(out=dstT[:, 1, sl], in_=p2[:, 0:128], func=AF.Copy, scale=scale), reads=[p2], writes=[dstT])
            pv = pb[4]
            for c in range(8):
                P.op('pe', lambda e, c=c, ht=ht: e.matmul(pv[:, 0:512], lhsT=ht[:, c, :], rhs=wret[:, c, 512:1024], start=(c == 0), stop=(c == 7)), reads=[wret, ht], writes=[pv])
            P.op('act', lambda e, t=t: e.copy(out=v_all[:, t, :], in_=pv[:, 0:512]), reads=[pv], writes=[v_all])
            if lat:
                pg = pb[5]
                for c in range(8):
                    P.op('pe', lambda e, c=c, ht=ht: e.matmul(pg[:, 0:512], lhsT=ht[:, c, :], rhs=wret[:, c, 1024:1536], start=(c == 0), stop=(c == 7)), reads=[wret, ht], writes=[pg])
                sg_ = sgb[t % 2]
                P.op('act', lambda e, sg_=sg_: e.activation(out=sg_[:], in_=pg[:, 0:512], func=AF.Silu), reads=[pg], writes=[sg_])
                P.dma('sp', lambda e, sg_=sg_, t=t: e.dma_start(out=sg_d[t * 128:(t + 1) * 128, :], in_=sg_[:]), reads=[sg_], writes=[sg_d])

        S = P.sb("S", [128, 2, 512], F32)
        Sb = P.sb("Sb", [128, 2, 512], BF16)
        kdT = P.sbn("kdT", [128, 2, 128], BF16, 2)
        qdT = P.sbn("qdT", [128, 2, 128], BF16, 2)
        kdtm = P.sbn("kdtm", [128, 256], BF16, 2)
        att = P.sbn("att", [128, 128], BF16, 2)
        ofb = P.sbn("ofb", [128, 512], F32, 2)
        osum = P.sbn("osum", [128, 512], F32, 2)
        sgl = P.sbn("sgl", [128, 512], BF16, 2)
        ogb = P.sbn("ogb", [128, 512], BF16, 2)
        junk = P.sb("junk", [128, 512], F32)
        ss = P.sbn("ss", [128, 1], F32, 2)
        rstd = P.sbn("rstd", [128, 1], F32, 2)
        onesg = P.sb("onesg", [128, 512], F32)
        P.op('dve', lambda e: e.memset(onesg[:], 1.0), writes=[onesg])
        for d in range(2):
            P.op('dve', lambda e: e.memset(S[:], 0.0), writes=[S])
            P.op('dve', lambda e: e.memset(Sb[:], 0.0), writes=[Sb])
            if d == 0:
                order = [NL, NL + 1] + list(range(NL))
            else:
                order = [NL + 1, NL] + list(range(NL - 1, -1, -1))
            mk_ = msk[:, 0:128] if d == 0 else msk[:, 128:256]
            for n_, t in enumerate(order):
                lat = t < NL
                sl = slice(t * 128, (t + 1) * 128)
                kd, qd, ktm, at = kdT[n_ % 2], qdT[n_ % 2], kdtm[n_ % 2], att[n_ % 2]
                for c in range(2):
                    P.op('dve', lambda e, c=c, kd=kd, sl=sl, d=d: e.tensor_tensor(out=kd[:, c, :], in0=kT_all[:, c, sl], in1=kdec[d][:], op=ALU.mult), reads=[kT_all, kdec[d]], writes=[kd])
                    if lat:
                        P.op('dve', lambda e, c=c, qd=qd, sl=sl, d=d: e.tensor_tensor(out=qd[:, c, :], in0=qT_all[:, c, sl], in1=qdec[d][:], op=ALU.mult), reads=[qT_all, qdec[d]], writes=[qd])
                for c in range(2):
                    P.op('pe', lambda e, c=c, kd=kd: e.transpose(out=psT[:, c, :], in_=kd[:, c, :], identity=C['ident'][:, :]), reads=[kd, C['ident']], writes=[psT])
                P.op('act', lambda e, ktm=ktm: e.copy(out=ktm[:, :], in_=psT[:, 0:2, :].rearrange("p c n -> p (c n)")), reads=[psT], writes=[ktm])
                if lat:
                    pa = pb[0]
                    for c in range(2):
                        P.op('pe', lambda e, c=c, kd=kd, qd=qd: e.matmul(pa[:, 0:128], lhsT=kd[:, c, :], rhs=qd[:, c, :], start=(c == 0), stop=(c == 1)), reads=[kd, qd], writes=[pa])
                    P.op('dve', lambda e, at=at, mk_=mk_: e.tensor_tensor(out=at[:], in0=pa[:, 0:128], in1=mk_, op=ALU.mult), reads=[pa, msk], writes=[at])
                    po = pb[1 + n_ % 2]
                    P.op('pe', lambda e, at=at, po=po, t=t: e.matmul(po[:, 0:512], lhsT=at[:, :], rhs=v_all[:, t, :], start=True, stop=False), reads=[at, v_all], writes=[po])
                    for c in range(2):
                        P.op('pe', lambda e, c=c, qd=qd, po=po: e.matmul(po[:, 0:512], lhsT=qd[:, c, :], rhs=Sb[:, c, :], start=False, stop=(c == 1)), reads=[qd, Sb], writes=[po])
                for c in range(2):
                    pu = pb[3 + c]
                    P.op('pe', lambda e, c=c, ktm=ktm, pu=pu, t=t: e.matmul(pu[:, 0:512], lhsT=ktm[:, c * 128:(c + 1) * 128], rhs=v_all[:, t, :], start=True, stop=True), reads=[ktm, v_all], writes=[pu])
                    P.op('dve', lambda e, c=c, pu=pu: e.tensor_tensor(out=S[:, c, :], in0=pu[:, 0:512], in1=S[:, c, :], op=ALU.add), reads=[pu, S], writes=[S])
                    P.op('dve', lambda e, c=c, d=d: e.tensor_scalar(out=S[:, c, :], in0=S[:, c, :], scalar1=c128[:, d:d + 1], scalar2=None, op0=ALU.mult), reads=[S, c128], writes=[S])
                    P.op('act', lambda e, c=c: e.copy(out=Sb[:, c, :], in_=S[:, c, :]), reads=[S], writes=[Sb])
                if lat:
                    if d == 0:
                        o_ = ofb[n_ % 2]
                        P.op('act', lambda e, o_=o_, po=po: e.copy(out=o_[:], in_=po[:, 0:512]), reads=[po], writes=[o_])
                        P.dma('sp', lambda e, o_=o_, sl=sl: e.dma_start(out=of_d[sl, :], in_=o_[:]), reads=[o_], writes=[of_d])
                    else:
                        o_ = ofb[n_ % 2]
                        sg_ = sgl[n_ % 2]
                        os_ = osum[n_ % 2]
                        og_ = ogb[n_ % 2]
                        P.dma('sp', lambda e, o_=o_, sl=sl: e.dma_start(out=o_[:], in_=of_d[sl, :]), reads=[of_d], writes=[o_])
                        P.dma('sp', lambda e, sg_=sg_, sl=sl: e.dma_start(out=sg_[:], in_=sg_d[sl, :]), reads=[sg_d], writes=[sg_])
                        P.op('dve', lambda e, o_=o_, po=po, os_=os_: e.tensor_tensor(out=os_[:], in0=po[:, 0:512], in1=o_[:], op=ALU.add), reads=[po, o_], writes=[os_])
                        emit_norm_mod(P, os_, os_[:], sg_, None, og_, og_[:], junk, ss[n_ % 2], rstd[n_ % 2], D=512)
                        P.dma('sp', lambda e, og_=og_, sl=sl: e.dma_start(out=og_d[sl, :], in_=og_[:]), reads=[og_], writes=[og_d])
        P.finish()
    return nc


NEGV = -1.0e30


def mk_condT(cb, cctx):
    arr = np.empty((128, 16), np.float32)
    arr[:, 0:8] = cb.reshape(8, 128).T
    arr[:, 8:16] = cctx.reshape(8, 128).T
    return np.ascontiguousarray(arr)


def mk_R(rpb_h):
    j = np.arange(64)[:, None]
    kc = np.arange(64)[None, :]
    cstart = np.clip(j - 8, 0, 48)
    ok = (kc >= cstart) & (kc < cstart + 16)
    dcol = np.clip(kc - j, -15, 15) + 15
    R = np.empty((64, 15, 64), np.float32)
    for dr in range(15):
        R[:, dr, :] = np.where(ok, rpb_h[dr][dcol], np.float32(NEGV))
    return R.reshape(64, 960)


def p1_inmaps(I):
    maps = []
    w_in = I["ab_w_in"][0]
    for core in range(8):
        b, hg = core // 4, core % 4
        xs = np.concatenate([I["x"][b], I["ctx"][b]], 0)
        R2 = np.stack([mk_R(I["na_rpb"][0][2 * hg]), mk_R(I["na_rpb"][0][2 * hg + 1])], 1)
        Rb = np.concatenate([R2, R2], 0)
        m = dict(
            xs=xs,
            condT=mk_condT(I["c"][b], I["c_ctx"]),
            adaw=I["ada_w"][0], adab=I["ada_b"][0][None],
            n1g=I["norm1_g"][0][None],
            wqkv=np.concatenate([w_in[:, hg * 128:(hg + 1) * 128], w_in[:, 512 + hg * 128:512 + (hg + 1) * 128],
                                 w_in[:, 1024 + hg * 128:1024 + (hg + 1) * 128]], 1),
            Rb=Rb,
            xq=np.concatenate([I["x"][b, hg * 2048:(hg + 1) * 2048], I["ctx"][b]], 0),
            wug=w_in[:, 1536:2560],
            sgug=I["sgu_norm_g"][0][None],
            swT=np.transpose(I["sgu_w"][0], (2, 0, 1)),
            sbT=I["sgu_b"][0].T,
        )
        maps.append({k: np.ascontiguousarray(v) for k, v in m.items()})
    return maps


def p2_inmaps(I, maps1, res1):
    maps = []
    for core in range(8):
        b, q = core // 4, core % 4
        rows = np.r_[q * 2048:(q + 1) * 2048, 8192:8448]
        a = np.concatenate([res1[b * 4 + hg]["a_out"][rows] for hg in range(4)], 1)
        ab = np.concatenate([a, res1[core]["bsg_out"]], 1)
        m = dict(x_tok=maps1[core]["xq"], ab=ab, condT=maps1[core]["condT"], adaw=I["ada_w"][0], adab=I["ada_b"][0][None],
                 n2g=I["norm2_g"][0][None], wout=I["ab_w_out"][0], wr=I["moe_router"][0])
        maps.append({k: np.ascontiguousarray(v) for k, v in m.items()})
    return maps


def consts_moe():
    k = np.arange(128)
    G = (k[:, None] // 32 == k[None, :] // 32).astype(np.float32)
    Tri = (k[:, None] < k[None, :]).astype(np.float32)
    return G, Tri


def gather_tok(res2, key, b, has_ctx, rows_lat=2048):
    parts = [res2[b * 4 + q][key][0:rows_lat] for q in range(4)]
    if has_ctx:
        parts.append(res2[b * 4][key][rows_lat:rows_lat + 256])
    return np.concatenate(parts, 0)


def p3_inmaps(I, layer, res2, has_ctx):
    G, Tri = consts_moe()
    affT = [np.concatenate([res2[b * 4 + q]["affT_out"][:, 0:2048] for q in range(4)] + ([res2[b * 4]["affT_out"][:, 2048:2304]] if has_ctx else []), 1) for b in range(2)]
    h2_all = np.stack([gather_tok(res2, "h2_out", b, has_ctx) for b in range(2)], 0)
    maps = []
    for i in range(8):
        aff4 = np.stack([affT[b][2 * i + el] for b in range(2) for el in range(2)], 0)
        m = dict(aff4=aff4, h2_all=h2_all, wg=I["moe_w_gate"][layer][2 * i:2 * i + 2], wu=I["moe_w_up"][layer][2 * i:2 * i + 2],
                 wd=I["moe_w_down"][layer][2 * i:2 * i + 2], Gc=G, Tric=Tri)
        maps.append({k: np.ascontiguousarray(v) for k, v in m.items()})
    return maps


def p4_inmaps(I, layer, res2, res3, has_ctx, last):
    NS = 1056 if has_ctx else 1024
    NJ = 66 if has_ctx else 64
    ntl = 18 if has_ctx else 16
    maps = []
    for core in range(8):
        b, q = core // 4, core % 4
        ye_b = np.concatenate([res3[e // 2]["ye_out"][b * 2 + e % 2] for e in range(16)], 0)
        js = list(range(q * 16, q * 16 + 16)) + ([64, 65] if has_ctx else [])
        d16 = np.empty((128, ntl, 16), np.float32)
        for e in range(16):
            dd = res3[e // 2]["dest_out"].reshape(128, 4, NJ)[:, b * 2 + e % 2, :]
            d16[:, :, e] = dd[:, js]
        offs = np.empty((128, 16), np.float32)
        for e in range(16):
            offs[:, e] = (e - (b * 2 + e % 2)) * NS
        m = dict(x1=res2[core]["x1_out"], affT=res2[core]["affT_out"], dest16=d16.reshape(128, ntl * 16), offs=offs, ye_b=ye_b,
                 condT=mk_condT(I["c"][b], I["c_ctx"]), adaw=I["ada_w"][layer], adab=I["ada_b"][layer][None])
        if last:
            m["fng"] = I["final_norm_g"][None]
        else:
            m["adaw_n"] = I["ada_w"][layer + 1]; m["adab_n"] = I["ada_b"][layer + 1][None]; m["n1g_n"] = I["norm1_g"][layer + 1][None]
        maps.append({k: np.ascontiguousarray(v) for k, v in m.items()})
    return maps


def rope_tables():
    inv = (np.float32(1.0) / (np.float32(10000.0) ** (np.arange(0, 128, 2, dtype=np.float32) / np.float32(128)))).astype(np.float32)
    t = np.arange(8192)
    r = (t // 64).astype(np.float32)
    col = (t % 64).astype(np.float32)
    ang = np.concatenate([r[:, None] * inv, col[:, None] * inv], -1).astype(np.float32)
    return np.ascontiguousarray(np.cos(ang).T.astype(np.float32)), np.ascontiguousarray(np.sin(ang).T.astype(np.float32))


def p5_inmaps(I, res4):
    cosT, sinT = rope_tables()
    i_ = np.arange(128, dtype=np.float32)
    iota = np.broadcast_to(np.concatenate([i_, 127 - i_])[None, :], (128, 256))
    k = np.arange(128)
    msk = np.concatenate([(k[None, :] >= k[:, None]), (k[None, :] <= k[:, None])], 1).astype(np.float32)
    w_in = I["ret_w_in"][0]
    maps = []
    for core in range(8):
        b, h = core // 4, core % 4
        hl = gather_tok(res4, "hn_out", b, True)
        wret = np.concatenate([w_in[:, h * 256:(h + 1) * 256], w_in[:, 1024 + h * 256:1024 + (h + 1) * 256],
                               w_in[:, 2048 + h * 512:2048 + (h + 1) * 512], w_in[:, 4096 + h * 512:4096 + (h + 1) * 512]], 1)
        dlog = np.broadcast_to(I["ret_decay_logit"][0][:, h][None, :], (128, 2))
        m = dict(hl=hl, wret=wret, cosT=cosT, sinT=sinT, dlog=dlog, iota=iota, msk=msk)
        maps.append({kk: np.ascontiguousarray(v) for kk, v in m.items()})
    return maps


def p6_inmaps(I, res4, res5):
    maps = []
    for core in range(8):
        b, q = core // 4, core % 4
        ab = np.concatenate([res5[b * 4 + h]["og_out"][q * 2048:(q + 1) * 2048] for h in range(4)], 1)
        m = dict(x_tok=res4[core]["x2_out"][0:2048], ab=ab, condT=mk_condT(I["c"][b], I["c_ctx"]), adaw=I["ada_w"][1], adab=I["ada_b"][1][None],
                 n2g=I["norm2_g"][1][None], wout=I["ret_w_out"][0], wr=I["moe_router"][1])
        maps.append({k: np.ascontiguousarray(v) for k, v in m.items()})
    return maps


_CORES = list(range(8))


def _run(nc, maps):
    return run_bass_kernel_spmd(nc, maps, core_ids=_CORES).results


def kernel(**inputs):
    I = {k: np.asarray(v) for k, v in inputs.items()}
    m1 = p1_inmaps(I)
    r1 = _run(build_p1(), m1)
    m2 = p2_inmaps(I, m1, r1)
    r2 = _run(build_p2(1024, 18, 16), m2)
    del r1
    m3 = p3_inmaps(I, 0, r2, True)
    r3 = _run(build_p3(True), m3)
    del m3
    m4 = p4_inmaps(I, 0, r2, r3, True, False)
    r4 = _run(build_p4(18, 16, 1056, False), m4)
    del m4, r3, r2, m2, m1
    m5 = p5_inmaps(I, r4)
    r5 = _run(build_p5(), m5)
    del m5
    m6 = p6_inmaps(I, r4, r5)
    r6 = _run(build_p2(2048, 16, 16), m6)
    del m6, r5, r4
    m7 = p3_inmaps(I, 1, r6, False)
    r7 = _run(build_p3(False), m7)
    del m7
    m8 = p4_inmaps(I, 1, r6, r7, False, True)
    r8 = _run(build_p4(16, 16, 1024, True), m8)
    out = np.empty((2, 8192, 1024), np.float32)
    for core in range(8):
        b, q = core // 4, core % 4
        out[b, q * 2048:(q + 1) * 2048] = np.asarray(r8[core]["y_out"])
    return out
```
